# Optimizing a Trainium2 kernel written in Bass

```python
import jax, jax.numpy as jnp
from jax import lax
import numpy as np

D_MODEL = 2048
BATCH = 4
SEQ = 2048
DEPTH = 1

RET_HEADS = 8
RET_DK = 128
RET_DV = 128
HG_HEADS = 8
HG_DK = 128
HG_DV = 128
SECTION_SIZES = (RET_HEADS * RET_DK, RET_HEADS * RET_DK, RET_HEADS * RET_DV, RET_HEADS * RET_DV,
                 HG_HEADS * HG_DK, HG_HEADS * HG_DK, HG_HEADS * HG_DV, HG_HEADS * HG_DV)
IN_WIDTH = sum(SECTION_SIZES)
MIX_WIDTH = RET_HEADS * RET_DV + HG_HEADS * HG_DV
CHUNK = 64
ROPE_BASE = 10000.0
N_EXPERTS = 256
TOP_K = 8
N_GROUPS = 8
TOPK_GROUPS = 4
EXPERT_FF = 512
SHARED_FF = 512
ROUTED_SCALE = 2.5
EXPERT_BLOCK = 128
NORM_EPS = 1e-5
DEEPNORM_ALPHA = (2.0 * DEPTH) ** 0.25
DEEPNORM_BETA = (8.0 * DEPTH) ** -0.25

kernel_name = "hybrid_retention_hgrn2_moe_deepnorm"


def layer_norm(x, gain, bias):
    xf = x.astype(jnp.float32)
    mu = jnp.mean(xf, axis=-1, keepdims=True)
    var = jnp.mean(jnp.square(xf - mu), axis=-1, keepdims=True)
    return ((xf - mu) * lax.rsqrt(var + NORM_EPS) * gain.astype(jnp.float32)
            + bias.astype(jnp.float32)).astype(x.dtype)


def head_norm(o, subtract_mean):
    if subtract_mean:
        o = o - jnp.mean(o, axis=-1, keepdims=True)
    return o * lax.rsqrt(jnp.mean(jnp.square(o), axis=-1, keepdims=True) + NORM_EPS)


def rotary(t, pos):
    half = t.shape[-1] // 2
    inv_freq = ROPE_BASE ** (-jnp.arange(half, dtype=jnp.float32) / half)
    ang = pos[:, None] * inv_freq[None, :]
    cos = jnp.cos(ang)[None, :, None, :].astype(t.dtype)
    sin = jnp.sin(ang)[None, :, None, :].astype(t.dtype)
    t1, t2 = t[..., :half], t[..., half:]
    return jnp.concatenate([t1 * cos - t2 * sin, t1 * sin + t2 * cos], axis=-1)


def chunked_linear_recurrence(q, k, v, log_a):
    b_, h_, t_, dk = q.shape
    dv = v.shape[-1]
    da = log_a.shape[-1]
    n_chunks = t_ // CHUNK

    def to_chunks(t):
        return jnp.moveaxis(t.astype(jnp.float32).reshape(b_, h_, n_chunks, CHUNK, t.shape[-1]), 2, 0)

    causal = jnp.tril(jnp.ones((CHUNK, CHUNK), dtype=bool))

    def step(state, inp):
        qc, kc, vc, lc = inp
        b = jnp.cumsum(lc, axis=-2)
        b_last = b[..., -1:, :]
        o_inter = jnp.einsum('bhtk,bhkv->bhtv', qc * jnp.exp(b), state)
        if da == 1:
            diff = b[..., :, None, 0] - b[..., None, :, 0]
            decay = jnp.exp(jnp.where(causal, diff, -jnp.inf))
            scores = jnp.einsum('bhtk,bhsk->bhts', qc, kc) * decay
        else:
            diff = b[..., :, None, :] - b[..., None, :, :]
            decay = jnp.exp(jnp.where(causal[..., None], diff, -jnp.inf))
            scores = jnp.einsum('bhtk,bhsk,bhtsk->bhts', qc, kc, decay)
        o = o_inter + jnp.einsum('bhts,bhsv->bhtv', scores, vc)
        new_state = (jnp.exp(jnp.swapaxes(b_last, -1, -2)) * state
                     + jnp.einsum('bhsk,bhsv->bhkv', kc * jnp.exp(b_last - b), vc))
        return new_state, o

    s0 = jnp.zeros((b_, h_, dk, dv), jnp.float32)
    _, o = lax.scan(step, s0, (to_chunks(q), to_chunks(k), to_chunks(v), to_chunks(log_a)))
    return jnp.moveaxis(o, 0, 2).reshape(b_, h_, t_, dv)


def split_heads(t, n_heads):
    b_, s_, w = t.shape
    return t.reshape(b_, s_, n_heads, w // n_heads)


def hybrid_mixer(x, pos, ret_log_decay, lower_bound, w_in, ret_gn_gain, hgrn_norm_gain, w_out):
    b_, s_, _ = x.shape
    proj = x @ w_in
    split_at = [int(i) for i in np.cumsum(SECTION_SIZES)[:-1]]
    rq, rk, rv, rg, hq, hf, hi, hg = jnp.split(proj, split_at, axis=-1)

    rq = rotary(split_heads(rq, RET_HEADS), pos)
    rk = rotary(split_heads(rk, RET_HEADS), pos) * (RET_DK ** -0.5)
    rv = split_heads(rv, RET_HEADS)
    log_a_ret = jnp.broadcast_to(ret_log_decay[None, :, None, None], (b_, RET_HEADS, s_, 1))
    ro = chunked_linear_recurrence(rq.transpose(0, 2, 1, 3), rk.transpose(0, 2, 1, 3),
                                   rv.transpose(0, 2, 1, 3), log_a_ret)
    ro = head_norm(ro.transpose(0, 2, 1, 3), True).reshape(b_, s_, RET_HEADS * RET_DV)
    ret_out = ro * ret_gn_gain.astype(jnp.float32) * jax.nn.silu(rg.astype(jnp.float32))

    lb = lower_bound[None, None, :]
    f = lb + (1.0 - lb) * jax.nn.sigmoid(hf.astype(jnp.float32))
    log_f = jnp.log(f)
    hk = 1.0 - f
    to_bhsd = lambda t: split_heads(t, HG_HEADS).transpose(0, 2, 1, 3)
    ho = chunked_linear_recurrence(to_bhsd(hq), to_bhsd(hk), to_bhsd(hi), to_bhsd(log_f))
    ho = head_norm(ho.transpose(0, 2, 1, 3), False).reshape(b_, s_, HG_HEADS * HG_DV)
    hg_out = ho * hgrn_norm_gain.astype(jnp.float32) * jax.nn.silu(hg.astype(jnp.float32))

    mixed = jnp.concatenate([ret_out, hg_out], axis=-1).astype(x.dtype)
    return mixed @ w_out


def moe_ffn(h, w_router, router_bias, w_gate, w_up, w_down, ws_gate, ws_up, ws_down):
    b_, s_, d = h.shape
    n_tok = b_ * s_
    hf = h.reshape(n_tok, d)
    scores = jax.nn.sigmoid((hf @ w_router).astype(jnp.float32))
    sel = scores + router_bias.astype(jnp.float32)
    grp = sel.reshape(n_tok, N_GROUPS, N_EXPERTS // N_GROUPS)
    group_score = jnp.sum(lax.top_k(grp, 2)[0], axis=-1)
    _, gidx = lax.top_k(group_score, TOPK_GROUPS)
    gmask = jnp.any(gidx[..., None] == jnp.arange(N_GROUPS), axis=-2)
    emask = jnp.repeat(gmask, N_EXPERTS // N_GROUPS, axis=-1)
    _, eidx = lax.top_k(jnp.where(emask, sel, -jnp.inf), TOP_K)
    gate = jnp.take_along_axis(scores, eidx, axis=-1)
    gate = gate / jnp.sum(gate, axis=-1, keepdims=True) * ROUTED_SCALE

    n_assign = n_tok * TOP_K
    e_flat = eidx.reshape(n_assign)
    tok_flat = jnp.arange(n_assign, dtype=jnp.int32) // TOP_K
    w_flat = gate.reshape(n_assign)
    order = jnp.argsort(e_flat)
    e_sorted = e_flat[order]
    counts = jnp.bincount(e_flat, length=N_EXPERTS)
    padded = (counts + EXPERT_BLOCK - 1) // EXPERT_BLOCK * EXPERT_BLOCK
    ends = jnp.cumsum(padded)
    pad_start = ends - padded
    seg_start = jnp.cumsum(counts) - counts
    dest = pad_start[e_sorted] + jnp.arange(n_assign) - seg_start[e_sorted]
    n_blocks = -(-n_assign // EXPERT_BLOCK) + N_EXPERTS
    n_rows = n_blocks * EXPERT_BLOCK
    row_tok = jnp.full((n_rows,), n_tok, jnp.int32).at[dest].set(tok_flat[order])
    row_w = jnp.zeros((n_rows,), jnp.float32).at[dest].set(w_flat[order])
    block_e = jnp.minimum(jnp.searchsorted(ends, jnp.arange(n_blocks) * EXPERT_BLOCK, side='right'),
                          N_EXPERTS - 1)
    x_rows = jnp.concatenate([hf, jnp.zeros((1, d), hf.dtype)], axis=0)[row_tok]
    x_rows = x_rows.reshape(n_blocks, EXPERT_BLOCK, d)

    def expert_block(args):
        xb, e = args
        return (jax.nn.silu(xb @ w_gate[e]) * (xb @ w_up[e])) @ w_down[e]

    y_rows = lax.map(expert_block, (x_rows, block_e)).reshape(n_rows, d)
    routed = jnp.zeros((n_tok + 1, d), h.dtype).at[row_tok].add(
        y_rows * row_w[:, None].astype(h.dtype))[:n_tok]
    shared = (jax.nn.silu(hf @ ws_gate) * (hf @ ws_up)) @ ws_down
    return (routed + shared).reshape(b_, s_, d)


def setup_inputs(seed: int = 0) -> dict:
    key = jax.random.key(seed)
    ks = jax.random.split(key, 20)
    f32 = jnp.float32
    beta = DEEPNORM_BETA
    col_scale = jnp.concatenate([
        jnp.full((n,), beta if i in (2, 6) else 1.0, f32) for i, n in enumerate(SECTION_SIZES)])
    nrm = lambda k, shape: jax.random.normal(k, shape, f32)
    return {
        "x": nrm(ks[0], (BATCH, SEQ, D_MODEL)),
        "w_in": nrm(ks[1], (DEPTH, D_MODEL, IN_WIDTH)) * (D_MODEL ** -0.5) * col_scale,
        "ret_gn_gain": 1.0 + 0.02 * nrm(ks[2], (DEPTH, RET_HEADS * RET_DV)),
        "hgrn_lb_logits": 0.5 * nrm(ks[3], (DEPTH + 1, HG_HEADS * HG_DK)),
        "hgrn_norm_gain": 1.0 + 0.02 * nrm(ks[4], (DEPTH, HG_HEADS * HG_DV)),
        "w_out": nrm(ks[5], (DEPTH, MIX_WIDTH, D_MODEL)) * (MIX_WIDTH ** -0.5) * beta,
        "ln1_gain": 1.0 + 0.02 * nrm(ks[6], (DEPTH, D_MODEL)),
        "ln1_bias": 0.02 * nrm(ks[7], (DEPTH, D_MODEL)),
        "w_router": nrm(ks[8], (DEPTH, D_MODEL, N_EXPERTS)) * (D_MODEL ** -0.5),
        "router_bias": 0.01 * nrm(ks[9], (DEPTH, N_EXPERTS)),
        "w_gate": nrm(ks[10], (DEPTH, N_EXPERTS, D_MODEL, EXPERT_FF)) * (D_MODEL ** -0.5),
        "w_up": nrm(ks[11], (DEPTH, N_EXPERTS, D_MODEL, EXPERT_FF)) * (D_MODEL ** -0.5) * beta,
        "w_down": nrm(ks[12], (DEPTH, N_EXPERTS, EXPERT_FF, D_MODEL)) * (EXPERT_FF ** -0.5) * beta,
        "ws_gate": nrm(ks[13], (DEPTH, D_MODEL, SHARED_FF)) * (D_MODEL ** -0.5),
        "ws_up": nrm(ks[14], (DEPTH, D_MODEL, SHARED_FF)) * (D_MODEL ** -0.5) * beta,
        "ws_down": nrm(ks[15], (DEPTH, SHARED_FF, D_MODEL)) * (SHARED_FF ** -0.5) * beta,
        "ln2_gain": 1.0 + 0.02 * nrm(ks[16], (DEPTH, D_MODEL)),
        "ln2_bias": 0.02 * nrm(ks[17], (DEPTH, D_MODEL)),
    }


def reference(x, w_in, ret_gn_gain, hgrn_lb_logits, hgrn_norm_gain, w_out, ln1_gain, ln1_bias,
              w_router, router_bias, w_gate, w_up, w_down, ws_gate, ws_up, ws_down,
              ln2_gain, ln2_bias):
    seq = x.shape[1]
    pos = jnp.arange(seq, dtype=jnp.float32)
    ret_log_decay = jnp.log(1.0 - jnp.power(2.0, -5.0 - jnp.arange(RET_HEADS, dtype=jnp.float32)))
    lower_bounds = jnp.cumsum(jax.nn.softmax(hgrn_lb_logits.astype(jnp.float32), axis=0), axis=0)
    for l in range(DEPTH):
        mix = hybrid_mixer(x, pos, ret_log_decay, lower_bounds[l], w_in[l], ret_gn_gain[l],
                           hgrn_norm_gain[l], w_out[l])
        x = layer_norm(DEEPNORM_ALPHA * x + mix, ln1_gain[l], ln1_bias[l])
        ffn = moe_ffn(x, w_router[l], router_bias[l], w_gate[l], w_up[l], w_down[l],
                      ws_gate[l], ws_up[l], ws_down[l])
        x = layer_norm(DEEPNORM_ALPHA * x + ffn, ln2_gain[l], ln2_bias[l])
    return x
```

```python
import math
import numpy as np
import concourse.bass as bass
import concourse.mybir as mybir
from concourse.bass_utils import run_bass_kernel_spmd

F32 = mybir.dt.float32
F32R = mybir.dt.float32r
BF16 = mybir.dt.bfloat16
I32 = mybir.dt.int32
U32 = mybir.dt.uint32
U8 = mybir.dt.uint8
AF = mybir.ActivationFunctionType
ALU = mybir.AluOpType
AX = mybir.AxisListType

D = 2048
NT = 16
NOWN = 8
NE = 256
CAP = 64
NSLOT = NE * CAP
EPS = 1e-5
ALPHA = 2.0 ** 0.25
ENGS = ("pe", "act", "dve", "pool", "sp")
NDMA = 40
NDMA_SW = 24


class Buf:
    def __init__(self):
        self.ws = []
        self.rs = []
        self.base = None


class Sched:
    def __init__(self):
        self.ops = {e: [] for e in ENGS}
        self.cnt = {e: 0 for e in ENGS}
        self.dma_val = [0] * NDMA
        self.dma_last = [None] * NDMA
        self.dma_rr = 0
        self.dma_rr_sw = 0

    def op(self, eng, fn, reads=(), writes=(), pwrites=(), dma=False, after=()):
        deps = set()
        for b in after:
            deps.update(b.ws)
            deps.update(b.rs)
        for b in reads:
            deps.update(b.ws)
        for b in writes:
            deps.update(b.ws)
            deps.update(b.rs)
        for b in pwrites:
            deps.update(b.rs)
            if b.base is not None:
                deps.add(b.base)
        if dma:
            if eng == "pool":
                d = self.dma_rr_sw
                self.dma_rr_sw = (self.dma_rr_sw + 1) % NDMA_SW
            else:
                d = NDMA_SW + self.dma_rr
                self.dma_rr = (self.dma_rr + 1) % (NDMA - NDMA_SW)
            if self.dma_last[d] is not None:
                deps.add(self.dma_last[d])
            self.dma_val[d] += 16
            tok = ("d", d, self.dma_val[d])
            self.dma_last[d] = tok
        else:
            self.cnt[eng] += 1
            tok = ("e", eng, self.cnt[eng])
        self.ops[eng].append((fn, deps, tok))
        for b in reads:
            b.rs.append(tok)
        for b in writes:
            b.ws = [tok]
            b.rs = []
            b.base = tok
        for b in pwrites:
            b.ws.append(tok)
            b.rs = []
        return tok

    def emit(self, nc, block, esem, dsem):
        def make(engname):
            def body(e):
                seen = {}
                for fn, deps, tok in self.ops[engname]:
                    need = {}
                    for (k, key, val) in deps:
                        if k == "e" and key == engname and engname == "pe":
                            continue
                        kk = (k, key)
                        if seen.get(kk, 0) >= val:
                            continue
                        if need.get(kk, 0) < val:
                            need[kk] = val
                    for (k, key), val in need.items():
                        e.wait_ge(esem[key] if k == "e" else dsem[key], val)
                        seen[(k, key)] = val
                    if fn is None:
                        continue
                    ins = fn(e)
                    if tok[0] == "e":
                        ins.then_inc(esem[engname], 1)
                    else:
                        ins.then_inc(dsem[tok[1]], 16)
            return body
        block.tensor(make("pe"))
        block.scalar(make("act"))
        block.vector(make("dve"))
        block.gpsimd(make("pool"))
        block.sync(make("sp"))


def build(ne=NE, debug=False, stop_after=None, debug_heads=(0, 8)):
    nc = bass.Bass("TRN2", target_bir_lowering=False)
    S = Sched()

    def din(name, shape, dt=F32):
        return nc.dram_tensor(name, list(shape), dt, kind="ExternalInput").ap()

    xT_d = din("xT", [D, NT * 128])
    xown_d = din("xown", [NOWN * 128, D])
    posb_d = din("posb", [128, 1])
    win_d = din("w_in_h", [16, D, 512])
    wout_d = din("w_out", [D, D])
    rgain_d = din("ret_gain", [1024])
    hgain_d = din("hg_gain", [1024])
    lbl_d = din("lb_logits", [2, 1024])
    ln_d = din("ln_par", [4, D])
    wr_d = din("w_router", [D, NE])
    rb_d = din("router_bias", [NE])
    wg_d = din("w_gate", [ne, D, 512])
    wu_d = din("w_up", [ne, D, 512])
    wd_d = din("w_down", [ne, 512, D])
    wsg_d = din("ws_gate", [D, 512])
    wsu_d = din("ws_up", [D, 512])
    wsd_d = din("ws_down", [512, D])
    out_d = nc.dram_tensor("out", [NOWN * 128, D], F32, kind="ExternalOutput").ap()
    dbg = {}
    if debug:
        dbg["mixT"] = nc.dram_tensor("dbg_mixT", [128, 16 * 1024], BF16, kind="ExternalOutput").ap()
        dbg["h"] = nc.dram_tensor("dbg_h", [NOWN * 128, D], F32, kind="ExternalOutput").ap()
        dbg["rt"] = nc.dram_tensor("dbg_rt", [NOWN * 128, 16], F32, kind="ExternalOutput").ap()
        dbg["acc"] = nc.dram_tensor("dbg_acc", [NOWN * 128, D], F32, kind="ExternalOutput").ap()
    xbuf_d = nc.dram_tensor("xbuf", [NSLOT, D], BF16).ap()
    ybuf_d = nc.dram_tensor("ybuf", [NSLOT, D], F32).ap()
    accb_d = nc.dram_tensor("accbuf", [NOWN * 128, D], F32).ap()

    from contextlib import ExitStack
    es = ExitStack()
    ARENA = 200 * 1024
    arena = es.enter_context(nc.sbuf_tensor("arena", [128, ARENA], U8))
    ps = [es.enter_context(nc.psum_tensor(f"ps{i}", [128, 2048], U8)) for i in range(8)]
    esem = {e: es.enter_context(nc.semaphore(f"es_{e}")) for e in ENGS}
    dsem = [es.enter_context(nc.semaphore(f"ds_{i}")) for i in range(NDMA)]

    def sb(off, shape, dt):
        size = {F32: 4, BF16: 2, I32: 4, U32: 4, F32R: 4}[dt]
        n = int(np.prod(shape[1:]))
        ap = arena[0:shape[0], off:off + n * size].bitcast(dt)
        if len(shape) == 3:
            ap = ap.rearrange("p (a b) -> p a b", a=shape[1])
        return ap

    def pv(bank, off, shape, dt):
        size = {F32: 4, BF16: 2}[dt]
        n = int(np.prod(shape[1:]))
        ap = ps[bank][0:shape[0], off:off + n * size].bitcast(dt)
        if len(shape) == 3:
            ap = ap.rearrange("p (a b) -> p a b", a=shape[1])
        return ap

    class Alloc:
        def __init__(self, base, limit):
            self.o = base
            self.limit = limit

        def take(self, nbytes):
            o = self.o
            self.o += (nbytes + 63) // 64 * 64
            assert self.o <= self.limit, (self.o, self.limit)
            return o

    KB = 1024
    A = Alloc(0, 40 * KB)
    ident_bf = sb(A.take(256), [128, 128], BF16)
    ident_f = sb(A.take(512), [128, 128], F32)
    iota_p = sb(A.take(4), [128, 1], F32)
    iota_f = sb(A.take(1024), [128, 256], F32)
    mask_c = sb(A.take(512), [128, 128], F32)
    mask_b = sb(A.take(512), [128, 128], F32)
    ublk = sb(A.take(512), [128, 128], F32)
    oblk = sb(A.take(512), [128, 128], F32)
    ustrict = sb(A.take(512), [128, 128], F32)
    ones_f = sb(A.take(512), [128, 128], F32)
    cos2 = sb(A.take(NT * 512), [128, NT, 128], F32)
    sin2 = sb(A.take(NT * 512), [128, NT, 128], F32)
    qdec = sb(A.take(32), [128, 8], F32)
    kdec = sb(A.take(32), [128, 8], F32)
    lb_bc = sb(A.take(4096), [128, 1024], F32)
    oml_bc = sb(A.take(4096), [128, 1024], F32)
    gain_bc = sb(A.take(8192), [128, 2048], F32)
    posb_t = sb(A.take(4), [128, 1], F32)
    negpi = sb(A.take(4), [128, 1], F32)
    eps_t = sb(A.take(4), [128, 1], F32)
    selAB = sb(A.take(8), [128, 2], F32)
    ublk_bf = sb(A.take(256), [128, 128], BF16)
    oblk_bf = sb(A.take(256), [128, 128], BF16)
    selAB_bf = sb(A.take(4), [128, 2], BF16)
    ustrict_bf = sb(A.take(256), [128, 128], BF16)
    ones_bf = sb(A.take(256), [128, 128], BF16)
    B_const = Buf()

    g = lambda f: f

    tmpA = Alloc(40 * KB, 120 * KB)
    t_i = sb(tmpA.take(8192), [128, 2048], F32)
    t_j = sb(tmpA.take(8192), [128, 2048], F32)
    t_k = sb(tmpA.take(8192), [128, 2048], F32)
    B_t = Buf()
    S.op("pool", lambda e: e.iota(iota_f, pattern=[[1, 256]], base=0, channel_multiplier=0,
                                  allow_small_or_imprecise_dtypes=True), writes=[B_const])
    S.op("pool", lambda e: e.iota(iota_p, pattern=[[0, 1]], base=0, channel_multiplier=1,
                                  allow_small_or_imprecise_dtypes=True), writes=[B_const])
    S.op("dve", lambda e: e.memset(ones_f, 1.0), writes=[B_const])
    S.op("dve", lambda e: e.memset(negpi, -math.pi), writes=[B_const])
    S.op("dve", lambda e: e.memset(eps_t, EPS), writes=[B_const])
    S.op("dve", lambda e: e.tensor_scalar(t_i[:, 0:128], iota_f[:, 0:128], iota_p[:, 0:1], None, ALU.subtract),
         reads=[B_const], writes=[B_t])
    S.op("dve", lambda e: e.tensor_single_scalar(mask_c, t_i[:, 0:128], 0.0, ALU.is_ge), reads=[B_t], writes=[B_const])
    S.op("dve", lambda e: e.tensor_single_scalar(ustrict, t_i[:, 0:128], 0.0, ALU.is_gt), reads=[B_t], writes=[B_const])
    S.op("dve", lambda e: e.tensor_single_scalar(ident_f, t_i[:, 0:128], 0.0, ALU.is_equal), reads=[B_t], writes=[B_const])
    S.op("dve", lambda e: e.tensor_copy(ident_bf, ident_f), reads=[B_const], writes=[B_const])
    S.op("dve", lambda e: e.tensor_single_scalar(t_j[:, 0:128], iota_f[:, 0:128], 64.0, ALU.is_ge), reads=[B_const], writes=[B_t])
    S.op("dve", lambda e: e.tensor_single_scalar(t_j[:, 128:129], iota_p[:, 0:1], 64.0, ALU.is_ge), reads=[B_const], writes=[B_t])
    S.op("dve", lambda e: e.tensor_scalar(oblk, t_j[:, 0:128], t_j[:, 128:129], None, ALU.is_equal), reads=[B_t], writes=[B_const])
    S.op("dve", lambda e: e.tensor_copy(selAB[:, 1:2], t_j[:, 128:129]), reads=[B_t], writes=[B_const])
    S.op("dve", lambda e: e.tensor_scalar(selAB[:, 0:1], t_j[:, 128:129], -1.0, 1.0, ALU.mult, ALU.add), reads=[B_t], writes=[B_const])
    S.op("dve", lambda e: e.tensor_tensor(mask_b, mask_c, oblk, ALU.mult), reads=[B_const], writes=[B_const])
    S.op("dve", lambda e: e.tensor_copy(ublk, mask_b), reads=[B_const], writes=[B_const])
    S.op("dve", lambda e: e.tensor_copy(ublk_bf, mask_b), reads=[B_const], writes=[B_const])
    S.op("dve", lambda e: e.tensor_copy(oblk_bf, oblk), reads=[B_const], writes=[B_const])
    S.op("dve", lambda e: e.tensor_copy(selAB_bf, selAB), reads=[B_const], writes=[B_const])
    S.op("dve", lambda e: e.tensor_copy(ustrict_bf, ustrict), reads=[B_const], writes=[B_const])
    S.op("dve", lambda e: e.tensor_copy(ones_bf, ones_f), reads=[B_const], writes=[B_const])
    for h in range(8):
        lg = math.log(1.0 - 2.0 ** (-5.0 - h))
        S.op("act", lambda e, h=h, lg=lg: e.activation(qdec[:, h:h + 1], iota_p[:, 0:1], AF.Exp, scale=lg),
             reads=[B_const], writes=[B_const])
    S.op("dve", lambda e: e.reciprocal(kdec, qdec), reads=[B_const], writes=[B_const])
    for h in range(8):
        lg = math.log(1.0 - 2.0 ** (-5.0 - h))
        S.op("dve", lambda e, h=h, lg=lg: e.tensor_scalar(qdec[:, h:h + 1], qdec[:, h:h + 1], math.exp(lg), None, ALU.mult),
             reads=[B_const], writes=[B_const])
        S.op("dve", lambda e, h=h, lg=lg: e.tensor_scalar(kdec[:, h:h + 1], kdec[:, h:h + 1], math.exp(-lg) * (128.0 ** -0.5), None, ALU.mult),
             reads=[B_const], writes=[B_const])
    bc = lambda ap1d: ap1d.partition_broadcast(128)
    S.op("sp", lambda e: e.dma_start(out=posb_t, in_=posb_d), writes=[B_const], dma=True)
    S.op("sp", lambda e: e.dma_start(out=gain_bc[:, 0:1024], in_=bc(rgain_d)), pwrites=[B_const], dma=True)
    S.op("sp", lambda e: e.dma_start(out=gain_bc[:, 1024:2048], in_=bc(hgain_d)), pwrites=[B_const], dma=True)
    S.op("sp", lambda e: e.dma_start(out=t_i[:, 0:1024], in_=bc(lbl_d[0])), writes=[B_t], dma=True)
    S.op("sp", lambda e: e.dma_start(out=t_j[:, 0:1024], in_=bc(lbl_d[1])), pwrites=[B_t], dma=True)
    S.op("dve", lambda e: e.tensor_tensor(t_k[:, 0:1024], t_i[:, 0:1024], t_j[:, 0:1024], ALU.subtract), reads=[B_t], pwrites=[B_t])
    S.op("act", lambda e: e.activation(lb_bc, t_k[:, 0:1024], AF.Sigmoid), reads=[B_t], pwrites=[B_const])
    S.op("dve", lambda e: e.tensor_scalar(oml_bc, lb_bc, -1.0, 1.0, ALU.mult, ALU.add), reads=[B_const], pwrites=[B_const])
    invf = t_i[:, 1024:1088]
    S.op("act", lambda e: e.activation(invf, iota_f[:, 0:64], AF.Exp, scale=-math.log(10000.0) / 64.0),
         reads=[B_const, B_t], pwrites=[B_t])
    TWO_PI = 2.0 * math.pi
    ni_t = sb(40 * KB + 3 * 8192, [128, 128], I32)
    for t in range(NT):
        pc = t_j[:, 1100 + t:1101 + t]
        ang = t_k[:, 1024:1152]
        uu = t_k[:, 1152:1280]
        nf = t_k[:, 1280:1408]
        rr = t_k[:, 1408:1536]
        mm = t_k[:, 1536:1664]
        sc_ = t_k[:, 1664:1792]
        S.op("dve", lambda e, pc=pc, t=t: e.tensor_scalar(pc, iota_p[:, 0:1], posb_t[:, 0:1], float(128 * t), ALU.add, ALU.add),
             reads=[B_const, B_t], writes=[B_t])
        S.op("dve", lambda e, pc=pc: e.tensor_single_scalar(pc, pc, 0.0, ALU.max), reads=[B_t], writes=[B_t])
        S.op("dve", lambda e, pc=pc: e.tensor_scalar(ang[:, 0:64], invf, pc, None, ALU.mult), reads=[B_t], writes=[B_t])
        S.op("dve", lambda e: e.tensor_scalar(ang[:, 64:128], ang[:, 0:64], math.pi / 2.0, None, ALU.add), reads=[B_t], writes=[B_t])
        S.op("dve", lambda e: e.tensor_scalar(uu, ang, 1.0 / TWO_PI, None, ALU.mult), reads=[B_t], writes=[B_t])
        S.op("dve", lambda e: e.tensor_copy(ni_t, uu), reads=[B_t], writes=[B_t])
        S.op("dve", lambda e: e.tensor_copy(nf, ni_t), reads=[B_t], writes=[B_t])
        S.op("dve", lambda e: e.scalar_tensor_tensor(rr, nf, -TWO_PI, ang, ALU.mult, ALU.add), reads=[B_t], writes=[B_t])
        S.op("dve", lambda e: e.tensor_single_scalar(mm, rr, math.pi, ALU.is_gt), reads=[B_t], writes=[B_t])
        S.op("dve", lambda e: e.scalar_tensor_tensor(rr, mm, -TWO_PI, rr, ALU.mult, ALU.add), reads=[B_t], writes=[B_t])
        S.op("dve", lambda e: e.tensor_single_scalar(mm, rr, -math.pi, ALU.is_lt), reads=[B_t], writes=[B_t])
        S.op("dve", lambda e: e.scalar_tensor_tensor(rr, mm, TWO_PI, rr, ALU.mult, ALU.add), reads=[B_t], writes=[B_t])
        S.op("act", lambda e: e.activation(sc_, rr, AF.Sin), reads=[B_t], writes=[B_t])
        S.op("dve", lambda e, t=t: e.tensor_scalar(sin2[:, t, 0:64], sc_[:, 0:64], -1.0, None, ALU.mult), reads=[B_t], pwrites=[B_const])
        S.op("dve", lambda e, t=t: e.tensor_copy(sin2[:, t, 64:128], sc_[:, 0:64]), reads=[B_t], pwrites=[B_const])
        S.op("dve", lambda e, t=t: e.tensor_copy(cos2[:, t, 0:64], sc_[:, 64:128]), reads=[B_t], pwrites=[B_const])
        S.op("dve", lambda e, t=t: e.tensor_copy(cos2[:, t, 64:128], sc_[:, 64:128]), reads=[B_t], pwrites=[B_const])

    if stop_after == "const":
        dh = dbg["h"]
        S.op("sp", lambda e: e.dma_start(out=dh[0:128, 0:2048], in_=cos2.rearrange("p a b -> p (a b)")), reads=[B_const], dma=True)
        S.op("sp", lambda e: e.dma_start(out=dh[128:256, 0:2048], in_=sin2.rearrange("p a b -> p (a b)")), reads=[B_const], dma=True)
        S.op("sp", lambda e: e.dma_start(out=dh[256:384, 0:1024], in_=lb_bc), reads=[B_const], dma=True)
        S.op("sp", lambda e: e.dma_start(out=dh[384:512, 0:2048], in_=gain_bc), reads=[B_const], dma=True)
        S.op("sp", lambda e: e.dma_start(out=dh[512:640, 0:128], in_=mask_b), reads=[B_const], dma=True)
        S.op("sp", lambda e: e.dma_start(out=dh[640:768, 0:128], in_=ident_f), reads=[B_const], dma=True)
        S.op("sp", lambda e: e.dma_start(out=dh[768:896, 0:8], in_=qdec), reads=[B_const], dma=True)
        S.op("sp", lambda e: e.dma_start(out=dh[896:1024, 0:8], in_=kdec), reads=[B_const], dma=True)
        return finish(nc, S, es, esem, dsem)
    M0 = 40 * KB
    xT_bf = sb(M0, [128, 16, NT * 128], BF16)
    win_bf = [sb(M0 + 64 * KB + i * 16 * KB, [128, 16, 512], BF16) for i in range(2)]
    mixT = sb(M0 + 96 * KB, [128, 16, NOWN * 128], BF16)
    WK = Alloc(M0 + 128 * KB, 200 * KB)
    B_xT = Buf(); B_win = [Buf(), Buf()]; B_mixT = Buf()
    for k in range(16):
        S.op("pool", lambda e, k=k: e.dma_start(out=xT_bf[:, k, :], in_=xT_d[k * 128:(k + 1) * 128, :]),
             after=[B_t, B_const], pwrites=[B_xT], dma=True)

    def wtile(shape, dt):
        size = {F32: 4, BF16: 2, I32: 4, U32: 4}[dt]
        return sb(WK.take(int(np.prod(shape[1:])) * size), shape, dt), Buf()

    NSET = 2
    W = []
    for s_ in range(NSET):
        d_ = {}
        for nm in ("qa", "qb", "ka", "kb", "sg", "on", "gs", "sig", "ff", "logf", "kk", "eb", "enb", "eblb", "ekb"):
            d_[nm] = wtile([128, 128], F32)
        for nm in ("lhi", "llo", "qd", "kdi", "kd2", "kd2A", "kd2B", "v", "qdT", "qdTA", "qdTB", "kdiT", "pT", "og"):
            d_[nm] = wtile([128, 128], BF16)
        d_["psb"] = wtile([128, 512], F32)
        d_["st6"] = wtile([128, 6], F32)
        d_["mv"] = wtile([128, 4], F32)
        d_["ebT"] = wtile([128, 4], F32)
        W.append(d_)
    ST = [(wtile([128, 128], F32), wtile([128, 128], BF16), wtile([128, 128], BF16)) for _ in range(2)]

    P_ps = [pv(0, 0, [128, 512], F32), pv(1, 0, [128, 512], F32)]
    B_P = [Buf(), Buf()]
    Bk = [Buf() for _ in range(8)]
    b_ps = pv(2, 0, [128, 128], F32); B_bps = Bk[2]
    blb_ps = pv(2, 512, [128, 128], F32); B_blbps = Bk[2]
    blT_ps = pv(2, 1024, [128, 4], F32); B_blTps = Bk[2]
    qdT_ps = pv(3, 0, [128, 128], BF16); B_qdTps = Bk[3]
    kdiT_ps = pv(3, 256, [128, 128], BF16); B_kdiTps = Bk[3]
    ogT_ps = pv(3, 512, [128, 128], BF16); B_ogTps = Bk[3]
    sc_ps = pv(4, 0, [128, 128], F32); B_scps = Bk[4]
    o_ps = pv(5, 0, [128, 128], F32); B_ops = Bk[5]
    dSa_ps = pv(6, 0, [128, 128], F32); B_dSa = Bk[6]
    dSb_ps = pv(6, 512, [128, 128], F32); B_dSb = Bk[6]

    if debug:
        S.op("dve", lambda e: e.memset(mixT.rearrange("p a b -> p (a b)"), 0.0), after=[B_t, B_const], writes=[B_mixT])
    steps = [(hb, t, 0) for hb in range(16) for t in range(NT)]

    def load_win(hb, bi):
        for q in range(4):
            S.op("pool", lambda e, hb=hb, q=q, bi=bi: e.dma_start(
                out=win_bf[bi][:, 4 * q:4 * q + 4, :],
                in_=win_d[hb, 512 * q:512 * (q + 1), :].rearrange("(k p) f -> p k f", p=128)),
                writes=[B_win[bi]] if q == 0 else [], pwrites=[] if q == 0 else [B_win[bi]], dma=True)

    def proj(i):
        hb, t, sl = steps[i]
        pi = i % 2
        c0, c1 = (0, 512) if t >= NT - NOWN else (128, 384)
        for k in range(16):
            S.op("pe", lambda e, hb=hb, t=t, k=k, pi=pi, sl=sl, c0=c0, c1=c1: e.matmul(
                P_ps[pi][:, c0:c1], lhsT=xT_bf[:, k, t * 128:(t + 1) * 128], rhs=win_bf[sl][:, k, c0:c1],
                start=(k == 0), stop=(k == 15)),
                reads=[B_xT, B_win[sl]], writes=[B_P[pi]] if k == 0 else [], pwrites=[] if k == 0 else [B_P[pi]])

    def rotary(P, off, t, a, Ba, b, Bb, BP):
        S.op("dve", lambda e: e.tensor_tensor(a, P[:, off:off + 128], cos2[:, t, :], ALU.mult), reads=[BP, B_const], writes=[Ba])
        S.op("dve", lambda e: e.tensor_tensor(b[:, 0:64], P[:, off + 64:off + 128], sin2[:, t, 0:64], ALU.mult), reads=[BP, B_const], writes=[Bb])
        S.op("dve", lambda e: e.tensor_tensor(b[:, 64:128], P[:, off:off + 64], sin2[:, t, 64:128], ALU.mult), reads=[BP, B_const], pwrites=[Bb])
        S.op("dve", lambda e: e.tensor_tensor(a, a, b, ALU.add), reads=[Bb], writes=[Ba])

    def transpose_to(src, Bsrc, pst, Bpst, dst, Bdst, dst_p=False):
        S.op("pe", lambda e: e.transpose(pst, src, ident_bf), reads=[Bsrc, B_const], writes=[Bpst])
        S.op("act", lambda e: e.copy(dst, pst), reads=[Bpst], writes=[] if dst_p else [Bdst], pwrites=[Bdst] if dst_p else [])

    def head_out(w, hb, t, own_i, with_mean):
        on, Bon = w["on"]; gs, Bgs = w["gs"]; sg, Bsg = w["sg"]; og, Bog = w["og"]
        st6, Bst = w["st6"]; mv, Bmv = w["mv"]
        S.op("dve", lambda e: e.bn_stats(st6, o_ps), reads=[B_ops], writes=[Bst])
        S.op("dve", lambda e: e.bn_aggr(mv[:, 0:2], st6), reads=[Bst], writes=[Bmv])
        if with_mean:
            S.op("act", lambda e: e.activation(mv[:, 3:4], mv[:, 1:2], AF.Ln, bias=eps_t[:, 0:1]), reads=[Bmv, B_const], writes=[Bmv])
            S.op("act", lambda e: e.activation(mv[:, 2:3], mv[:, 3:4], AF.Exp, scale=-0.5), reads=[Bmv], writes=[Bmv])
            S.op("dve", lambda e: e.tensor_scalar(on, o_ps, mv[:, 0:1], mv[:, 2:3], ALU.subtract, ALU.mult), reads=[B_ops, Bmv], writes=[Bon])
        else:
            S.op("dve", lambda e: e.scalar_tensor_tensor(mv[:, 3:4], mv[:, 0:1], mv[:, 0:1], mv[:, 1:2], ALU.mult, ALU.add), reads=[Bmv], writes=[Bmv])
            S.op("act", lambda e: e.activation(mv[:, 3:4], mv[:, 3:4], AF.Ln, bias=eps_t[:, 0:1]), reads=[Bmv, B_const], writes=[Bmv])
            S.op("act", lambda e: e.activation(mv[:, 2:3], mv[:, 3:4], AF.Exp, scale=-0.5), reads=[Bmv], writes=[Bmv])
            S.op("dve", lambda e: e.tensor_scalar(on, o_ps, mv[:, 2:3], None, ALU.mult), reads=[B_ops, Bmv], writes=[Bon])
        S.op("dve", lambda e: e.tensor_tensor(gs, sg, gain_bc[:, hb * 128:(hb + 1) * 128], ALU.mult), reads=[Bsg, B_const], writes=[Bgs])
        S.op("dve", lambda e: e.tensor_tensor(og, on, gs, ALU.mult), reads=[Bon, Bgs], writes=[Bog])
        transpose_to(og, Bog, ogT_ps, B_ogTps, mixT[:, hb, own_i * 128:(own_i + 1) * 128], B_mixT, dst_p=True)

    import os as _os
    LVL = float(_os.environ.get('REC_LEVEL', '9'))

    def rec(i):
        hb, t, sl = steps[i]
        pi = i % 2
        w = W[i % NSET]
        P, BP = w["psb"]
        own = t >= NT - NOWN
        (T_f, B_T), (SbA, B_SbA), (SbB, B_SbB) = ST[sl]
        if own:
            S.op("act", lambda e: e.copy(P, P_ps[pi]), reads=[B_P[pi]], writes=[BP])
        else:
            S.op("act", lambda e: e.copy(P[:, 128:384], P_ps[pi][:, 128:384]), reads=[B_P[pi]], writes=[BP])
        if t == 0 or len(steps) == 1:
            S.op("dve", lambda e: e.memset(T_f, 0.0), writes=[B_T])
            S.op("dve", lambda e: e.memset(SbA, 0.0), writes=[B_SbA])
        v, Bv = w["v"]; kdi, Bkdi = w["kdi"]; qd, Bqd = w["qd"]
        qdT, BqdT = w["qdT"]; kdiT, BkdiT = w["kdiT"]; pT, BpT = w["pT"]; sg, Bsg = w["sg"]
        S.op("act", lambda e: e.copy(v, P[:, 256:384]), reads=[BP], writes=[Bv])
        if own and LVL >= 0.2:
            S.op("act", lambda e: e.activation(sg, P[:, 384:512], AF.Exp, scale=-1.0), reads=[BP], writes=[Bsg])
            S.op("dve", lambda e: e.tensor_scalar(sg, sg, 1.0, None, ALU.add), writes=[Bsg])
            S.op("dve", lambda e: e.reciprocal(sg, sg), writes=[Bsg])
            S.op("dve", lambda e: e.tensor_tensor(sg, sg, P[:, 384:512], ALU.mult), reads=[BP], writes=[Bsg])
        if hb < 8:
            h = hb
            g128 = (1.0 - 2.0 ** (-5.0 - h)) ** 128
            ka, Bka = w["ka"]; kb, Bkb = w["kb"]; qa, Bqa = w["qa"]; qb, Bqb = w["qb"]
            if LVL >= 0.4:
                rotary(P, 128, t, ka, Bka, kb, Bkb, BP)
            if LVL >= 0.6:
                S.op("act", lambda e: e.activation(kdi, ka, AF.Copy, scale=kdec[:, h:h + 1]), reads=[Bka, B_const], writes=[Bkdi])
            if own and LVL >= 2:
                rotary(P, 0, t, qa, Bqa, qb, Bqb, BP)
                S.op("act", lambda e: e.activation(qd, qa, AF.Copy, scale=qdec[:, h:h + 1]), reads=[Bqa, B_const], writes=[Bqd])
                transpose_to(qd, Bqd, qdT_ps, B_qdTps, qdT, BqdT)
                transpose_to(kdi, Bkdi, kdiT_ps, B_kdiTps, kdiT, BkdiT)
            if own and LVL >= 3:
                S.op("pe", lambda e: e.matmul(sc_ps, lhsT=kdiT, rhs=qdT, start=True, stop=True), reads=[BkdiT, BqdT], writes=[B_scps])
                S.op("dve", lambda e: e.tensor_tensor(pT, sc_ps, mask_c, ALU.mult), reads=[B_scps, B_const], writes=[BpT])
                S.op("pe", lambda e: e.matmul(o_ps, lhsT=pT, rhs=v, start=True, stop=False), reads=[BpT, Bv], writes=[B_ops])
                S.op("pe", lambda e: e.matmul(o_ps, lhsT=qdT, rhs=SbA, start=False, stop=True), reads=[BqdT, B_SbA], pwrites=[B_ops])
            if LVL >= 4:
                S.op("pe", lambda e: e.matmul(dSa_ps, lhsT=kdi, rhs=v, start=True, stop=True), reads=[Bkdi, Bv], writes=[B_dSa])
                S.op("dve", lambda e: e.tensor_tensor(T_f, T_f, dSa_ps, ALU.add), reads=[B_dSa], writes=[B_T])
                S.op("act", lambda e: e.activation(T_f, T_f, AF.Copy, scale=g128), writes=[B_T])
                S.op("act", lambda e: e.copy(SbA, T_f), reads=[B_T], writes=[B_SbA])
            if own and LVL >= 5:
                head_out(w, hb, t, t - (NT - NOWN), True)
        else:
            h = hb - 8
            sig, Bsig = w["sig"]; ff, Bff = w["ff"]; logf, Blogf = w["logf"]; kk, Bkk = w["kk"]
            eb, Beb = w["eb"]; enb, Benb = w["enb"]; eblb, Beblb = w["eblb"]; ekb, Bekb = w["ekb"]
            kd2, Bkd2 = w["kd2"]; ebT, BebT = w["ebT"]
            if LVL >= 0.2:
                S.op("act", lambda e: e.activation(sig, P[:, 128:256], AF.Exp, scale=-1.0), reads=[BP], writes=[Bsig])
                S.op("dve", lambda e: e.tensor_scalar(sig, sig, 1.0, None, ALU.add), writes=[Bsig])
                S.op("dve", lambda e: e.reciprocal(sig, sig), writes=[Bsig])
            if LVL >= 0.2:
                S.op("dve", lambda e: e.tensor_tensor(ff, sig, oml_bc[:, h * 128:(h + 1) * 128], ALU.mult), reads=[Bsig, B_const], writes=[Bff])
            if LVL >= 0.2:
                S.op("dve", lambda e: e.tensor_tensor(ff, ff, lb_bc[:, h * 128:(h + 1) * 128], ALU.add), reads=[B_const], writes=[Bff])
            if LVL >= 0.2:
                S.op("act", lambda e: e.activation(logf, ff, AF.Ln), reads=[Bff], writes=[Blogf])
            if LVL >= 0.2:
                S.op("dve", lambda e: e.tensor_scalar(kk, ff, -1.0, 1.0, ALU.mult, ALU.add), reads=[Bff], writes=[Bkk])
            lhi, Blhi = w["lhi"]; llo, Bllo = w["llo"]
            if LVL >= 0.4:
                S.op("act", lambda e: e.copy(lhi, logf), reads=[Blogf], writes=[Blhi])
                S.op("act", lambda e: e.copy(sig, lhi), reads=[Blhi], writes=[Bsig])
                S.op("dve", lambda e: e.tensor_tensor(llo, logf, sig, ALU.subtract), reads=[Blogf, Bsig], writes=[Bllo])
                S.op("pe", lambda e: e.matmul(b_ps, lhsT=ublk_bf, rhs=lhi, start=True, stop=False), reads=[Blhi, B_const], writes=[B_bps])
                S.op("pe", lambda e: e.matmul(b_ps, lhsT=ublk_bf, rhs=llo, start=False, stop=True), reads=[Bllo, B_const], pwrites=[B_bps])
                S.op("pe", lambda e: e.matmul(blb_ps, lhsT=oblk_bf, rhs=lhi, start=True, stop=False), reads=[Blhi, B_const], pwrites=[B_blbps])
                S.op("pe", lambda e: e.matmul(blb_ps, lhsT=oblk_bf, rhs=llo, start=False, stop=True), reads=[Bllo, B_const], pwrites=[B_blbps])
            if LVL >= 0.6:
                S.op("pe", lambda e: e.matmul(blT_ps[:, 0:2], lhsT=lhi, rhs=selAB_bf, start=True, stop=False), reads=[Blhi, B_const], pwrites=[B_blTps])
                S.op("pe", lambda e: e.matmul(blT_ps[:, 0:2], lhsT=llo, rhs=selAB_bf, start=False, stop=True), reads=[Bllo, B_const], pwrites=[B_blTps])
            if LVL >= 0.4:
                S.op("act", lambda e: e.activation(enb, b_ps, AF.Exp, scale=-1.0), reads=[B_bps], writes=[Benb])
            if LVL >= 0.4:
                S.op("act", lambda e: e.activation(eblb, blb_ps, AF.Exp), reads=[B_blbps], writes=[Beblb])
            if LVL >= 0.6:
                S.op("act", lambda e: e.activation(ebT[:, 0:2], blT_ps[:, 0:2], AF.Exp), reads=[B_blTps], writes=[BebT])
            if LVL >= 0.8:
                S.op("dve", lambda e: e.tensor_tensor(ekb, eblb, enb, ALU.mult), reads=[Beblb, Benb], writes=[Bekb])
            if LVL >= 0.8:
                S.op("dve", lambda e: e.tensor_tensor(kd2, kk, ekb, ALU.mult), reads=[Bkk, Bekb], writes=[Bkd2])
            kd2A, Bkd2A = w["kd2A"]; kd2B, Bkd2B = w["kd2B"]; qdTA, BqdTA = w["qdTA"]; qdTB, BqdTB = w["qdTB"]
            if LVL >= 0.8:
                S.op("dve", lambda e: e.tensor_scalar(kd2A, kd2, selAB[:, 0:1], None, ALU.mult), reads=[Bkd2, B_const], writes=[Bkd2A])
            if LVL >= 0.8:
                S.op("dve", lambda e: e.tensor_scalar(kd2B, kd2, selAB[:, 1:2], None, ALU.mult), reads=[Bkd2, B_const], writes=[Bkd2B])
            if own and LVL >= 2:
                S.op("act", lambda e: e.activation(eb, b_ps, AF.Exp), reads=[B_bps], writes=[Beb])
                S.op("dve", lambda e: e.tensor_tensor(qd, P[:, 0:128], eb, ALU.mult), reads=[BP, Beb], writes=[Bqd])
                S.op("dve", lambda e: e.tensor_tensor(kdi, kk, enb, ALU.mult), reads=[Bkk, Benb], writes=[Bkdi])
                transpose_to(qd, Bqd, qdT_ps, B_qdTps, qdT, BqdT)
                transpose_to(kdi, Bkdi, kdiT_ps, B_kdiTps, kdiT, BkdiT)
                S.op("dve", lambda e: e.memset(qdTA, 0.0), writes=[BqdTA])
                S.op("dve", lambda e: e.memset(qdTB, 0.0), writes=[BqdTB])
                S.op("dve", lambda e: e.tensor_copy(qdTA[:, 0:64], qdT[:, 0:64]), reads=[BqdT], pwrites=[BqdTA])
                S.op("dve", lambda e: e.tensor_copy(qdTB[:, 64:128], qdT[:, 64:128]), reads=[BqdT], pwrites=[BqdTB])
                S.op("pe", lambda e: e.matmul(sc_ps, lhsT=kdiT, rhs=qdT, start=True, stop=True), reads=[BkdiT, BqdT], writes=[B_scps])
                S.op("dve", lambda e: e.tensor_tensor(pT, sc_ps, mask_b, ALU.mult), reads=[B_scps, B_const], writes=[BpT])
            if own and LVL >= 3:
                S.op("pe", lambda e: e.matmul(o_ps, lhsT=pT, rhs=v, start=True, stop=False), reads=[BpT, Bv], writes=[B_ops])
                S.op("pe", lambda e: e.matmul(o_ps, lhsT=qdTA, rhs=SbA, start=False, stop=False), reads=[BqdTA, B_SbA], pwrites=[B_ops])
            if LVL < 4:
                return
            S.op("pe", lambda e: e.matmul(dSa_ps, lhsT=kd2A, rhs=v, start=True, stop=True), reads=[Bkd2A, Bv], writes=[B_dSa])
            S.op("dve", lambda e: e.scalar_tensor_tensor(T_f, T_f, ebT[:, 0:1], dSa_ps, ALU.mult, ALU.add), reads=[B_dSa, BebT], writes=[B_T])
            S.op("act", lambda e: e.copy(SbB, T_f), reads=[B_T], writes=[B_SbB])
            if LVL < 5:
                return
            if own:
                S.op("pe", lambda e: e.matmul(o_ps, lhsT=qdTB, rhs=SbB, start=False, stop=True), reads=[BqdTB, B_SbB], pwrites=[B_ops])
            S.op("pe", lambda e: e.matmul(dSb_ps, lhsT=kd2B, rhs=v, start=True, stop=True), reads=[Bkd2B, Bv], writes=[B_dSb])
            S.op("dve", lambda e: e.scalar_tensor_tensor(T_f, T_f, ebT[:, 1:2], dSb_ps, ALU.mult, ALU.add), reads=[B_dSb, BebT], writes=[B_T])
            S.op("act", lambda e: e.copy(SbA, T_f), reads=[B_T], writes=[B_SbA])
            if own and LVL >= 6:
                head_out(w, hb, t, t - (NT - NOWN), False)

    if stop_after == "proj":
        steps[:] = [(hb, t, 0) for hb in debug_heads for t in range(NT)]
        load_win(steps[0][0], 0); proj(0)
        tmp = sb(WK.take(2048), [128, 512], F32); Btmp = Buf()
        S.op("act", lambda e: e.copy(tmp, P_ps[0]), reads=[B_P[0]], writes=[Btmp])
        S.op("sp", lambda e: e.dma_start(out=dbg["h"][0:128, 0:512], in_=tmp), reads=[Btmp], dma=True)
        return finish(nc, S, es, esem, dsem)
    if stop_after == "rec1":
        steps[:] = [(debug_heads[0], 8, 0)]
        load_win(steps[0][0], 0); proj(0); rec(0)
        w0 = W[0]
        dh = dbg["h"]
        tmp = sb(WK.take(2048), [128, 512], F32); Btmp = Buf()
        S.op("dve", lambda e: e.tensor_copy(tmp[:, 0:128], w0["kdi"][0]), reads=[w0["kdi"][1]], writes=[Btmp])
        S.op("dve", lambda e: e.tensor_copy(tmp[:, 128:256], w0["v"][0]), reads=[w0["v"][1]], pwrites=[Btmp])
        S.op("dve", lambda e: e.tensor_copy(tmp[:, 256:384], w0["qdT"][0]), reads=[w0["qdT"][1]], pwrites=[Btmp])
        S.op("dve", lambda e: e.tensor_copy(tmp[:, 384:512], w0["pT"][0]), reads=[w0["pT"][1]], pwrites=[Btmp])
        S.op("sp", lambda e: e.dma_start(out=dh[0:128, 0:512], in_=tmp), reads=[Btmp], dma=True)
        S.op("sp", lambda e: e.dma_start(out=dh[128:256, 0:128], in_=ST[0][0][0]), reads=[ST[0][0][1]], dma=True)
        S.op("sp", lambda e: e.dma_start(out=dh[256:384, 0:128], in_=w0["on"][0]), reads=[w0["on"][1]], dma=True)
        return finish(nc, S, es, esem, dsem)
    heads = list(range(16)) if not debug else list(debug_heads)
    steps[:] = []
    for pi_ in range(0, len(heads), 2):
        pair = heads[pi_:pi_ + 2]
        for t in range(NT):
            for sl, hb in enumerate(pair):
                steps.append((hb, t, sl))
    loaded = set()
    def ensure_win(i):
        hb, _, sl = steps[i]
        if hb not in loaded:
            load_win(hb, sl); loaded.add(hb)
    ensure_win(0); proj(0)
    for i in range(len(steps)):
        if i + 1 < len(steps):
            ensure_win(i + 1); proj(i + 1)
        rec(i)
    if stop_after == "mixer":
        S.op("sp", lambda e: e.dma_start(out=dbg["mixT"], in_=mixT.rearrange("p a b -> p (a b)")), reads=[B_mixT], dma=True)
        return finish(nc, S, es, esem, dsem)

    B_M = [B_xT, B_win[0], B_win[1], B_t] + [b for st_ in ST for (_, b) in st_] + [b for d_ in W for (_, b) in d_.values()]
    R0 = 40 * KB
    NRING = 8
    ring = [sb(R0 + i * 4096, [128, 2048], BF16) for i in range(NRING)]
    B_ring = [Buf() for _ in range(NRING)]
    xo = sb(R0 + 32 * KB, [128, 2048], F32); B_xo = Buf()
    ht = [sb(R0 + 40 * KB + i * 8192, [128, 2048], F32) for i in range(2)]; B_ht = [Buf(), Buf()]
    hbf = sb(R0 + 56 * KB, [128, 2048], BF16); B_hbf = Buf()
    hT = sb(R0 + 60 * KB, [128, 16, 128], BF16); B_hT = Buf()
    ln_g = sb(R0 + 64 * KB, [128, 2048], F32); ln_b = sb(R0 + 72 * KB, [128, 2048], F32); B_ln = Buf()
    wr_bf = sb(R0 + 80 * KB, [128, 16, 256], BF16); B_wr = Buf()
    em_bf = sb(R0 + 88 * KB, [128, 8, 256], BF16); B_em = Buf()
    Q = Alloc(168 * KB, 200 * KB)
    rb_bc = sb(Q.take(1024), [128, 256], F32); B_rb = Buf()
    slot_all = sb(Q.take(256), [128, 8, 8], I32); B_slot = Buf()
    wk_all = sb(Q.take(256), [128, 8, 8], F32); B_wk = Buf()
    rt = {}
    for nm in ("sc", "sel", "selm", "em", "gd", "csb"):
        rt[nm] = (sb(Q.take(1024), [128, 256], F32), Buf())
    gm = sb(Q.take(256), [128, 8, 8], F32); gsum = sb(Q.take(32), [128, 8], F32); m2 = sb(Q.take(32), [128, 8], F32)
    gmask = sb(Q.take(32), [128, 8], F32); pen = sb(Q.take(32), [128, 8], F32); m8 = sb(Q.take(32), [128, 8], F32)
    mk = sb(Q.take(32), [128, 8], F32); idxu = sb(Q.take(32), [128, 8], U32); idxf = sb(Q.take(32), [128, 8], F32)
    posf = sb(Q.take(32), [128, 8], F32); slotf = sb(Q.take(32), [128, 8], F32); okf = sb(Q.take(32), [128, 8], F32)
    ssum = sb(Q.take(8), [128, 2], F32); st24 = sb(Q.take(96), [128, 4, 6], F32); mvl = sb(Q.take(16), [128, 4], F32)
    B_r = Buf()
    a_f = sb(Q.take(2048), [128, 512], F32); B_af = Buf()
    a_bf = sb(Q.take(1024), [128, 512], BF16); B_abf = Buf()
    aT = sb(Q.take(1024), [128, 4, 128], BF16); B_aT = Buf()
    ysb = sb(Q.take(8192), [128, 2048], F32); B_ysb = Buf()
    xe = sb(Q.take(4096), [128, 2048], BF16); B_xe = Buf()
    xeT = sb(Q.take(2048), [128, 16, 64], BF16); B_xeT = Buf()
    zer = sb(Q.take(4096), [128, 2048], BF16); B_zer = Buf()

    B_WS = [B_xo, B_ht[0], B_ht[1], B_hbf, B_hT, B_ln, B_wr, B_em]
    Pb = [pv(i, 0, [128, 512], F32) for i in range(8)]
    Pbf = [pv(i, 0, [128, 1024], BF16) for i in range(8)]

    S.op("dve", lambda e: e.memset(zer, 0.0), after=B_M, writes=[B_zer])
    B_xbuf = Buf(); B_ybuf = Buf(); B_accb = Buf()
    nz = NSLOT // 128
    for i in range(0, ne * CAP // 128):
        S.op("sp", lambda e, i=i: e.dma_start(out=xbuf_d[i * 128:(i + 1) * 128, :], in_=zer), reads=[B_zer], pwrites=[B_xbuf], dma=True)
    S.op("dve", lambda e: e.memset(ysb, 0.0), after=B_M, writes=[B_ysb])
    S.op("pool", lambda e: e.dma_start(out=wr_bf[:, 0:8, :], in_=wr_d[0:1024, :].rearrange("(k p) f -> p k f", p=128)), after=B_M, writes=[B_wr], dma=True)
    S.op("pool", lambda e: e.dma_start(out=wr_bf[:, 8:16, :], in_=wr_d[1024:2048, :].rearrange("(k p) f -> p k f", p=128)), pwrites=[B_wr], dma=True)
    S.op("sp", lambda e: e.dma_start(out=rb_bc, in_=bc(rb_d)), after=B_M, writes=[B_rb], dma=True)
    S.op("sp", lambda e: e.dma_start(out=ln_g, in_=bc(ln_d[0])), after=B_M, writes=[B_ln], dma=True)
    S.op("sp", lambda e: e.dma_start(out=ln_b, in_=bc(ln_d[1])), pwrites=[B_ln], dma=True)

    piece_ctr = [0]
    ring_active = [NRING]
    NEXTRA = 16
    for i in range(NEXTRA):
        ring.append(sb(R0 + 32 * KB + i * 4096, [128, 2048], BF16))
        B_ring.append(Buf())
    first_use = set()

    def load_piece(src_ap, three_d):
        i = piece_ctr[0] % ring_active[0]
        piece_ctr[0] += 1
        dst = ring[i].rearrange("p (a b) -> p a b", a=4) if three_d else ring[i]
        aft = ()
        if i not in first_use:
            first_use.add(i)
            aft = B_M if i < NRING else B_WS
        S.op("pool", lambda e: e.dma_start(out=dst, in_=src_ap), after=aft, writes=[B_ring[i]], dma=True)
        return i

    def layer_norm(src, Bsrc, g_ap, b_ap):
        for q in range(4):
            S.op("dve", lambda e, q=q: e.bn_stats(st24[:, q, :], src[:, q * 512:(q + 1) * 512]), reads=[Bsrc], writes=[B_r] if q == 0 else [], pwrites=[] if q == 0 else [B_r])
        S.op("dve", lambda e: e.bn_aggr(mvl[:, 0:2], st24.rearrange("p a b -> p (a b)")), reads=[B_r], pwrites=[B_r])
        S.op("act", lambda e: e.activation(mvl[:, 3:4], mvl[:, 1:2], AF.Ln, bias=eps_t[:, 0:1]), reads=[B_r, B_const], pwrites=[B_r])
        S.op("act", lambda e: e.activation(mvl[:, 2:3], mvl[:, 3:4], AF.Exp, scale=-0.5), reads=[B_r], pwrites=[B_r])
        S.op("dve", lambda e: e.tensor_scalar(src, src, mvl[:, 0:1], mvl[:, 2:3], ALU.subtract, ALU.mult), reads=[B_r], writes=[Bsrc])
        S.op("dve", lambda e: e.tensor_tensor(src, src, g_ap, ALU.mult), reads=[B_ln], writes=[Bsrc])
        S.op("dve", lambda e: e.tensor_tensor(src, src, b_ap, ALU.add), reads=[B_ln], writes=[Bsrc])

    def ffn(xt_chunk, Bxt, M, g_ap, u_ap, d_ap):
        for which, w_ap, bank in (("g", g_ap, 0), ("u", u_ap, 1)):
            for q in range(4):
                ri = load_piece(w_ap[512 * q:512 * (q + 1), :].rearrange("(k p) f -> p k f", p=128), True)
                rv = ring[ri].rearrange("p (a b) -> p a b", a=4)
                for kk in range(4):
                    k = 4 * q + kk
                    S.op("pe", lambda e, rv=rv, kk=kk, k=k, bank=bank: e.matmul(Pb[bank][0:M, :], lhsT=xt_chunk(k), rhs=rv[:, kk, :], start=(k == 0), stop=(k == 15)),
                         reads=[Bxt, B_ring[ri]], writes=[Bk[bank]] if k == 0 else [], pwrites=[] if k == 0 else [Bk[bank]])
        S.op("act", lambda e: e.activation(a_f[0:M, :], Pb[0][0:M, :], AF.Silu), reads=[Bk[0]], writes=[B_af])
        S.op("dve", lambda e: e.tensor_tensor(a_bf[0:M, :], a_f[0:M, :], Pb[1][0:M, :], ALU.mult), reads=[B_af, Bk[1]], writes=[B_abf])
        for c in range(4):
            S.op("pe", lambda e, c=c: e.transpose(Pbf[2][:, c * 128:c * 128 + M], a_bf[0:M, c * 128:(c + 1) * 128], ident_bf[0:M, 0:M]),
                 reads=[B_abf, B_const], writes=[Bk[2]] if c == 0 else [], pwrites=[] if c == 0 else [Bk[2]])
        for c in range(4):
            S.op("act", lambda e, c=c: e.copy(aT[:, c, 0:M], Pbf[2][:, c * 128:c * 128 + M]), reads=[Bk[2]], writes=[B_aT] if c == 0 else [], pwrites=[] if c == 0 else [B_aT])
        for c in range(4):
            ri = load_piece(d_ap[c * 128:(c + 1) * 128, :], False)
            for nb in range(4):
                S.op("pe", lambda e, c=c, nb=nb, ri=ri: e.matmul(Pb[4 + nb][0:M, :], lhsT=aT[:, c, 0:M], rhs=ring[ri][:, nb * 512:(nb + 1) * 512], start=(c == 0), stop=(c == 3)),
                     reads=[B_aT, B_ring[ri]], writes=[Bk[4 + nb]] if c == 0 else [], pwrites=[] if c == 0 else [Bk[4 + nb]])

    BIGOOB = float(NSLOT + 4096)
    regc = {}

    def bcreg(e):
        if 'r' not in regc:
            regc['r'] = e.to_reg(ne * CAP - 1)
        return regc['r']

    for j in range(NOWN):
        hj = ht[j % 2]; Bhj = B_ht[j % 2]
        S.op("sp", lambda e, j=j: e.dma_start(out=xo, in_=xown_d[j * 128:(j + 1) * 128, :]), after=B_M if j == 0 else (), writes=[B_xo], dma=True)
        for hb in range(16):
            ri = load_piece(wout_d[hb * 128:(hb + 1) * 128, :], False)
            for nb in range(4):
                S.op("pe", lambda e, hb=hb, nb=nb, ri=ri, j=j: e.matmul(Pb[nb], lhsT=mixT[:, hb, j * 128:(j + 1) * 128], rhs=ring[ri][:, nb * 512:(nb + 1) * 512], start=(hb == 0), stop=(hb == 15)),
                     reads=[B_mixT, B_ring[ri]], writes=[Bk[nb]] if hb == 0 else [], pwrites=[] if hb == 0 else [Bk[nb]])
        for nb in range(4):
            S.op("dve", lambda e, nb=nb, hj=hj: e.scalar_tensor_tensor(hj[:, nb * 512:(nb + 1) * 512], xo[:, nb * 512:(nb + 1) * 512], ALPHA, Pb[nb], ALU.mult, ALU.add),
                 reads=[B_xo, Bk[nb]], after=B_M if j < 2 else (), writes=[Bhj] if nb == 0 else [], pwrites=[] if nb == 0 else [Bhj])
        layer_norm(hj, Bhj, ln_g, ln_b)
        if debug:
            S.op("sp", lambda e, j=j, hj=hj: e.dma_start(out=dbg["h"][j * 128:(j + 1) * 128, :], in_=hj), reads=[Bhj], dma=True)
        S.op("act", lambda e, hj=hj: e.copy(hbf, hj), reads=[Bhj], after=B_M if j == 0 else (), writes=[B_hbf])
        for half in range(2):
            for kk in range(8):
                k = half * 8 + kk
                S.op("pe", lambda e, k=k, kk=kk, half=half: e.transpose(Pbf[4 + half][:, kk * 128:(kk + 1) * 128], hbf[:, k * 128:(k + 1) * 128], ident_bf),
                     reads=[B_hbf, B_const], writes=[Bk[4 + half]] if kk == 0 else [], pwrites=[] if kk == 0 else [Bk[4 + half]])
            S.op("act", lambda e, half=half: e.copy(hT[:, half * 8:(half + 1) * 8, :].rearrange("p a b -> p (a b)"), Pbf[4 + half]), reads=[Bk[4 + half]],
                 after=B_M if j == 0 else (), writes=[B_hT] if half == 0 else [], pwrites=[] if half == 0 else [B_hT])
        for k in range(16):
            S.op("pe", lambda e, k=k: e.matmul(Pb[6][:, 0:256], lhsT=hT[:, k, :], rhs=wr_bf[:, k, :], start=(k == 0), stop=(k == 15)),
                 reads=[B_hT, B_wr], writes=[Bk[6]] if k == 0 else [], pwrites=[] if k == 0 else [Bk[6]])
        sc, Bsc = rt["sc"]; sel, Bsel = rt["sel"]; selm, Bselm = rt["selm"]; em, Bem = rt["em"]; gd, Bgd = rt["gd"]; csb, Bcsb = rt["csb"]
        S.op("act", lambda e: e.activation(sc, Pb[6][:, 0:256], AF.Sigmoid), reads=[Bk[6]], after=B_M if j == 0 else (), writes=[Bsc])
        S.op("dve", lambda e: e.tensor_tensor(sel, sc, rb_bc, ALU.add), reads=[Bsc, B_rb], writes=[Bsel])
        for g_ in range(8):
            S.op("dve", lambda e, g_=g_: e.max(gm[:, g_, :], sel[:, g_ * 32:(g_ + 1) * 32]), reads=[Bsel], writes=[B_r] if g_ == 0 else [], pwrites=[] if g_ == 0 else [B_r])
        S.op("dve", lambda e: e.tensor_tensor(gsum, gm[:, :, 0], gm[:, :, 1], ALU.add), reads=[B_r], pwrites=[B_r])
        S.op("dve", lambda e: e.max(m2, gsum), reads=[B_r], pwrites=[B_r])
        S.op("dve", lambda e: e.tensor_scalar(gmask, gsum, m2[:, 3:4], None, ALU.is_ge), reads=[B_r], pwrites=[B_r])
        S.op("dve", lambda e: e.tensor_scalar(pen, gmask, -1.0, 1e9, ALU.add, ALU.mult), reads=[B_r], pwrites=[B_r])
        for g_ in range(8):
            S.op("dve", lambda e, g_=g_: e.tensor_scalar(selm[:, g_ * 32:(g_ + 1) * 32], sel[:, g_ * 32:(g_ + 1) * 32], gmask[:, g_:g_ + 1], pen[:, g_:g_ + 1], ALU.mult, ALU.add),
                 reads=[Bsel, B_r], writes=[Bselm] if g_ == 0 else [], pwrites=[] if g_ == 0 else [Bselm])
        S.op("dve", lambda e: e.max(m8, selm), reads=[Bselm], pwrites=[B_r])
        S.op("dve", lambda e: e.tensor_scalar(em, selm, m8[:, 7:8], None, ALU.is_ge), reads=[Bselm, B_r], writes=[Bem])
        S.op("dve", lambda e, j=j: e.tensor_copy(em_bf[:, j, :], em), reads=[Bem], after=B_M if j == 0 else (), pwrites=[B_em])
        S.op("dve", lambda e: e.scalar_tensor_tensor(gd, sc, 1.0, em, ALU.mult, ALU.mult, accum_out=ssum[:, 0:1]), reads=[Bsc, Bem], writes=[Bgd], pwrites=[B_r])
        S.op("dve", lambda e: e.reciprocal(ssum[:, 1:2], ssum[:, 0:1]), reads=[B_r], pwrites=[B_r])
        S.op("dve", lambda e: e.tensor_scalar(gd, gd, ssum[:, 1:2], 2.5, ALU.mult, ALU.mult), reads=[B_r], writes=[Bgd])
        S.op("dve", lambda e: e.max(mk, gd), reads=[Bgd], pwrites=[B_r])
        S.op("dve", lambda e: e.max_index(idxu, mk, gd), reads=[Bgd, B_r], pwrites=[B_r])
        S.op("dve", lambda e: e.tensor_copy(idxf, idxu), reads=[B_r], pwrites=[B_r])
        S.op("pe", lambda e, j=j: e.matmul(Pb[7][:, 0:256], lhsT=ustrict_bf, rhs=em_bf[:, j, :], start=True, stop=(j == 0)), reads=[B_em, B_const], writes=[Bk[7]])
        for jj in range(j):
            S.op("pe", lambda e, jj=jj, j=j: e.matmul(Pb[7][:, 0:256], lhsT=ones_bf, rhs=em_bf[:, jj, :], start=False, stop=(jj == j - 1)), reads=[B_em, B_const], pwrites=[Bk[7]])
        S.op("act", lambda e: e.copy(csb, Pb[7][:, 0:256]), reads=[Bk[7]], writes=[Bcsb])
        for k in range(8):
            S.op("dve", lambda e, k=k: e.scalar_tensor_tensor(sel, iota_f, idxf[:, k:k + 1], csb, ALU.is_equal, ALU.mult, accum_out=posf[:, k:k + 1]),
                 reads=[Bcsb, B_r, B_const], writes=[Bsel], pwrites=[B_r])
        S.op("dve", lambda e: e.scalar_tensor_tensor(slotf, idxf, float(CAP), posf, ALU.mult, ALU.add), reads=[B_r], pwrites=[B_r])
        S.op("dve", lambda e: e.tensor_single_scalar(okf, posf, float(CAP), ALU.is_lt), reads=[B_r], pwrites=[B_r])
        if ne < NE:
            S.op("dve", lambda e: e.tensor_single_scalar(slotf, idxf, float(ne), ALU.is_lt), reads=[B_r], pwrites=[B_r])
            S.op("dve", lambda e: e.tensor_tensor(okf, okf, slotf, ALU.mult), reads=[B_r], pwrites=[B_r])
            S.op("dve", lambda e: e.scalar_tensor_tensor(slotf, idxf, float(CAP), posf, ALU.mult, ALU.add), reads=[B_r], pwrites=[B_r])
        S.op("dve", lambda e: e.scalar_tensor_tensor(slotf, slotf, -BIGOOB, okf, ALU.add, ALU.mult), reads=[B_r], pwrites=[B_r])
        S.op("dve", lambda e: e.tensor_scalar(slotf, slotf, BIGOOB, None, ALU.add), reads=[B_r], pwrites=[B_r])
        S.op("dve", lambda e, j=j: e.tensor_copy(slot_all[:, j, :], slotf), reads=[B_r], after=B_M if j == 0 else (), pwrites=[B_slot])
        S.op("dve", lambda e, j=j: e.tensor_tensor(wk_all[:, j, :], mk, okf, ALU.mult), reads=[B_r], after=B_M if j == 0 else (), pwrites=[B_wk])
        if debug:
            S.op("dve", lambda e: e.tensor_copy(sel[:, 0:8], idxf), reads=[B_r], writes=[Bsel])
            S.op("dve", lambda e, j=j: e.tensor_copy(sel[:, 8:16], wk_all[:, j, :]), reads=[B_wk], pwrites=[Bsel])
            S.op("sp", lambda e, j=j: e.dma_start(out=dbg["rt"][j * 128:(j + 1) * 128, :], in_=sel[:, 0:16]), reads=[Bsel], dma=True)
        for k in range(8):
            S.op("pool", lambda e, j=j, k=k: e.indirect_dma_start(out=xbuf_d[0:ne * CAP, :], out_offset=bass.IndirectOffsetOnAxis(ap=slot_all[:, j, k:k + 1], axis=0),
                                                              in_=hbf, in_offset=None, bounds_check=bcreg(e), oob_is_err=False),
                 reads=[B_hbf, B_slot], pwrites=[B_xbuf], after=[B_xbuf] if (j == 0 and k == 0) else (), dma=True)
        ffn(lambda k: hT[:, k, :], B_hT, 128, wsg_d, wsu_d, wsd_d)
        for nb in range(4):
            S.op("dve", lambda e, nb=nb, hj=hj: e.scalar_tensor_tensor(hj[:, nb * 512:(nb + 1) * 512], hj[:, nb * 512:(nb + 1) * 512], ALPHA, Pb[4 + nb], ALU.mult, ALU.add),
                 reads=[Bk[4 + nb]], writes=[Bhj])
        S.op("sp", lambda e, j=j, hj=hj: e.dma_start(out=accb_d[j * 128:(j + 1) * 128, :], in_=hj), reads=[Bhj], pwrites=[B_accb], dma=True)

    ring_active[0] = NRING + NEXTRA
    for ex in range(ne):
        S.op("sp", lambda e, ex=ex: e.dma_start(out=xe[0:CAP, :], in_=xbuf_d[ex * CAP:(ex + 1) * CAP, :]), reads=[B_xbuf], writes=[B_xe], dma=True)
        for k in range(16):
            S.op("pe", lambda e, k=k: e.transpose(Pbf[3][:, k * CAP:(k + 1) * CAP], xe[0:CAP, k * 128:(k + 1) * 128], ident_bf[0:CAP, 0:CAP]),
                 reads=[B_xe, B_const], writes=[Bk[3]] if k == 0 else [], pwrites=[] if k == 0 else [Bk[3]])
        S.op("act", lambda e: e.copy(xeT.rearrange("p a b -> p (a b)"), Pbf[3]), reads=[Bk[3]], writes=[B_xeT])
        ffn(lambda k: xeT[:, k, :], B_xeT, CAP, wg_d[ex], wu_d[ex], wd_d[ex])
        for nb in range(4):
            S.op("act", lambda e, nb=nb: e.copy(ysb[0:CAP, nb * 512:(nb + 1) * 512], Pb[4 + nb][0:CAP, :]), reads=[Bk[4 + nb]], writes=[B_ysb] if nb == 0 else [], pwrites=[] if nb == 0 else [B_ysb])
        S.op("sp", lambda e, ex=ex: e.dma_start(out=ybuf_d[ex * CAP:(ex + 1) * CAP, :], in_=ysb[0:CAP, :]), reads=[B_ysb], pwrites=[B_ybuf], dma=True)

    S.op("sp", lambda e: e.dma_start(out=ln_g, in_=bc(ln_d[2])), after=B_ring, writes=[B_ln], dma=True)
    S.op("sp", lambda e: e.dma_start(out=ln_b, in_=bc(ln_d[3])), pwrites=[B_ln], dma=True)
    yg = ring
    yg_t = [sb(R0 + i * 8192, [128, 2048], F32) for i in range(2)]
    B_yg = [Buf(), Buf()]
    for i in range(2):
        S.op("dve", lambda e, i=i: e.memset(yg_t[i], 0.0), after=B_ring, writes=[B_yg[i]])
    gi = 0
    for j in range(NOWN):
        hj = ht[j % 2]; Bhj = B_ht[j % 2]
        S.op("sp", lambda e, j=j, hj=hj: e.dma_start(out=hj, in_=accb_d[j * 128:(j + 1) * 128, :]), reads=[B_accb], after=B_ring if j < 2 else (), writes=[Bhj], dma=True)
        for k in range(8):
            t_ = yg_t[gi % 2]; Bt_ = B_yg[gi % 2]; gi += 1
            S.op("pool", lambda e, j=j, k=k, t_=t_: e.indirect_dma_start(out=t_, out_offset=None, in_=ybuf_d[0:ne * CAP, :],
                                                                        in_offset=bass.IndirectOffsetOnAxis(ap=slot_all[:, j, k:k + 1], axis=0),
                                                                        bounds_check=bcreg(e), oob_is_err=False),
                 reads=[B_ybuf, B_slot], writes=[Bt_], dma=True)
            S.op("dve", lambda e, j=j, k=k, t_=t_, hj=hj: e.scalar_tensor_tensor(hj, t_, wk_all[:, j, k:k + 1], hj, ALU.mult, ALU.add),
                 reads=[Bt_, B_wk], writes=[Bhj])
        if debug:
            S.op("sp", lambda e, j=j, hj=hj: e.dma_start(out=dbg["acc"][j * 128:(j + 1) * 128, :], in_=hj), reads=[Bhj], dma=True)
        layer_norm(hj, Bhj, ln_g, ln_b)
        S.op("sp", lambda e, j=j, hj=hj: e.dma_start(out=out_d[j * 128:(j + 1) * 128, :], in_=hj), reads=[Bhj], dma=True)
    return finish(nc, S, es, esem, dsem)


def finish(nc, S, es, esem, dsem):
    final = set(t for t in S.dma_last if t is not None)
    S.ops["sp"].append((None, final, None))
    with nc.Block() as block:
        S.emit(nc, block, esem, dsem)
    es.close()
    return nc


def host_layout(inputs, ne=NE):
    x = np.asarray(inputs["x"], np.float32)
    w_in = np.asarray(inputs["w_in"], np.float32)[0]
    secs = [w_in[:, i * 1024:(i + 1) * 1024] for i in range(8)]
    blocks = []
    for h in range(8):
        blocks.append(np.concatenate([secs[i][:, h * 128:(h + 1) * 128] for i in (0, 1, 2, 3)], axis=1))
    for h in range(8):
        blocks.append(np.concatenate([secs[i][:, h * 128:(h + 1) * 128] for i in (4, 5, 6, 7)], axis=1))
    w_in_h = np.ascontiguousarray(np.stack(blocks, 0))
    ln_par = np.ascontiguousarray(np.stack([inputs["ln1_gain"][0], inputs["ln1_bias"][0],
                                            inputs["ln2_gain"][0], inputs["ln2_bias"][0]], 0).astype(np.float32))
    shared = {
        "w_in_h": w_in_h,
        "w_out": np.ascontiguousarray(inputs["w_out"][0], np.float32),
        "ret_gain": np.ascontiguousarray(inputs["ret_gn_gain"][0], np.float32),
        "hg_gain": np.ascontiguousarray(inputs["hgrn_norm_gain"][0], np.float32),
        "lb_logits": np.ascontiguousarray(inputs["hgrn_lb_logits"], np.float32),
        "ln_par": ln_par,
        "w_router": np.ascontiguousarray(inputs["w_router"][0], np.float32),
        "router_bias": np.ascontiguousarray(inputs["router_bias"][0], np.float32),
        "w_gate": np.asarray(inputs["w_gate"][0][:ne], np.float32),
        "w_up": np.asarray(inputs["w_up"][0][:ne], np.float32),
        "w_down": np.asarray(inputs["w_down"][0][:ne], np.float32),
        "ws_gate": np.ascontiguousarray(inputs["ws_gate"][0], np.float32),
        "ws_up": np.ascontiguousarray(inputs["ws_up"][0], np.float32),
        "ws_down": np.ascontiguousarray(inputs["ws_down"][0], np.float32),
    }
    maps = []
    for c in range(8):
        b, th = c // 2, c % 2
        if th == 1:
            xT = np.ascontiguousarray(x[b].T)
        else:
            xT = np.ascontiguousarray(np.concatenate([np.zeros((D, 1024), np.float32), x[b, 0:1024].T], axis=1))
        m = dict(shared)
        m["xT"] = xT
        m["xown"] = np.ascontiguousarray(x[b, th * 1024:(th + 1) * 1024])
        m["posb"] = np.full((128, 1), 0.0 if th == 1 else -1024.0, np.float32)
        maps.append(m)
    return maps


def kernel(**inputs):
    maps = host_layout(inputs, ne=NE)
    nc = build(ne=NE, debug=False)
    res = run_bass_kernel_spmd(nc, maps, core_ids=list(range(8)))
    out = np.zeros((4, 2048, 2048), np.float32)
    for c in range(8):
        b, th = c // 2, c % 2
        out[b, th * 1024:(th + 1) * 1024] = np.asarray(res.results[c]["out"], np.float32)
    return out
```

```python
import math
import numpy as np
import concourse.bass as bass
import concourse.mybir as mybir
from concourse.bass_utils import run_bass_kernel_spmd

F32 = mybir.dt.float32
F32R = mybir.dt.float32r
BF16 = mybir.dt.bfloat16
I32 = mybir.dt.int32
U32 = mybir.dt.uint32
U8 = mybir.dt.uint8
AF = mybir.ActivationFunctionType
ALU = mybir.AluOpType
AX = mybir.AxisListType

D = 2048
NT = 16
NOWN = 8
NE = 256
CAP = 64
NSLOT = NE * CAP
EPS = 1e-5
ALPHA = 2.0 ** 0.25
ENGS = ("pe", "act", "dve", "pool", "sp")
NDMA = 40
NDMA_SW = 24


class Buf:
    def __init__(self):
        self.ws = []
        self.rs = []
        self.base = None


class Sched:
    def __init__(self):
        self.ops = {e: [] for e in ENGS}
        self.cnt = {e: 0 for e in ENGS}
        self.dma_val = [0] * NDMA
        self.dma_last = [None] * NDMA
        self.dma_rr = 0
        self.dma_rr_sw = 0

    def op(self, eng, fn, reads=(), writes=(), pwrites=(), dma=False, after=()):
        deps = set()
        for b in after:
            deps.update(b.ws)
            deps.update(b.rs)
        for b in reads:
            deps.update(b.ws)
        for b in writes:
            deps.update(b.ws)
            deps.update(b.rs)
        for b in pwrites:
            deps.update(b.rs)
            if b.base is not None:
                deps.add(b.base)
        if dma:
            if eng == "pool":
                d = self.dma_rr_sw
                self.dma_rr_sw = (self.dma_rr_sw + 1) % NDMA_SW
            else:
                d = NDMA_SW + self.dma_rr
                self.dma_rr = (self.dma_rr + 1) % (NDMA - NDMA_SW)
            if self.dma_last[d] is not None:
                deps.add(self.dma_last[d])
            self.dma_val[d] += 16
            tok = ("d", d, self.dma_val[d])
            self.dma_last[d] = tok
        else:
            self.cnt[eng] += 1
            tok = ("e", eng, self.cnt[eng])
        self.ops[eng].append((fn, deps, tok))
        for b in reads:
            b.rs.append(tok)
        for b in writes:
            b.ws = [tok]
            b.rs = []
            b.base = tok
        for b in pwrites:
            b.ws.append(tok)
            b.rs = []
        return tok

    def emit(self, nc, block, esem, dsem):
        def make(engname):
            def body(e):
                seen = {}
                for fn, deps, tok in self.ops[engname]:
                    need = {}
                    for (k, key, val) in deps:
                        if k == "e" and key == engname and engname == "pe":
                            continue
                        kk = (k, key)
                        if seen.get(kk, 0) >= val:
                            continue
                        if need.get(kk, 0) < val:
                            need[kk] = val
                    for (k, key), val in need.items():
                        e.wait_ge(esem[key] if k == "e" else dsem[key], val)
                        seen[(k, key)] = val
                    if fn is None:
                        continue
                    ins = fn(e)
                    if tok[0] == "e":
                        ins.then_inc(esem[engname], 1)
                    else:
                        ins.then_inc(dsem[tok[1]], 16)
            return body
        block.tensor(make("pe"))
        block.scalar(make("act"))
        block.vector(make("dve"))
        block.gpsimd(make("pool"))
        block.sync(make("sp"))


def build(ne=NE, debug=False, stop_after=None, debug_heads=(0, 8)):
    nc = bass.Bass("TRN2", target_bir_lowering=False)
    S = Sched()

    def din(name, shape, dt=F32):
        return nc.dram_tensor(name, list(shape), dt, kind="ExternalInput").ap()

    xT_d = din("xT", [D, NT * 128])
    xown_d = din("xown", [NOWN * 128, D])
    posb_d = din("posb", [128, 1])
    win_d = din("w_in_h", [16, D, 512])
    wout_d = din("w_out", [D, D])
    rgain_d = din("ret_gain", [1024])
    hgain_d = din("hg_gain", [1024])
    lbl_d = din("lb_logits", [2, 1024])
    ln_d = din("ln_par", [4, D])
    wr_d = din("w_router", [D, NE])
    rb_d = din("router_bias", [NE])
    wg_d = din("w_gate", [ne, D, 512])
    wu_d = din("w_up", [ne, D, 512])
    wd_d = din("w_down", [ne, 512, D])
    wsg_d = din("ws_gate", [D, 512])
    wsu_d = din("ws_up", [D, 512])
    wsd_d = din("ws_down", [512, D])
    out_d = nc.dram_tensor("out", [NOWN * 128, D], F32, kind="ExternalOutput").ap()
    dbg = {}
    if debug:
        dbg["mixT"] = nc.dram_tensor("dbg_mixT", [128, 16 * 1024], BF16, kind="ExternalOutput").ap()
        dbg["h"] = nc.dram_tensor("dbg_h", [NOWN * 128, D], F32, kind="ExternalOutput").ap()
        dbg["rt"] = nc.dram_tensor("dbg_rt", [NOWN * 128, 16], F32, kind="ExternalOutput").ap()
        dbg["acc"] = nc.dram_tensor("dbg_acc", [NOWN * 128, D], F32, kind="ExternalOutput").ap()
    xbuf_d = nc.dram_tensor("xbuf", [NSLOT, D], BF16).ap()
    ybuf_d = nc.dram_tensor("ybuf", [NSLOT, D], F32).ap()
    accb_d = nc.dram_tensor("accbuf", [NOWN * 128, D], F32).ap()

    from contextlib import ExitStack
    es = ExitStack()
    ARENA = 200 * 1024
    arena = es.enter_context(nc.sbuf_tensor("arena", [128, ARENA], U8))
    ps = [es.enter_context(nc.psum_tensor(f"ps{i}", [128, 2048], U8)) for i in range(8)]
    esem = {e: es.enter_context(nc.semaphore(f"es_{e}")) for e in ENGS}
    dsem = [es.enter_context(nc.semaphore(f"ds_{i}")) for i in range(NDMA)]

    def sb(off, shape, dt):
        size = {F32: 4, BF16: 2, I32: 4, U32: 4, F32R: 4}[dt]
        n = int(np.prod(shape[1:]))
        ap = arena[0:shape[0], off:off + n * size].bitcast(dt)
        if len(shape) == 3:
            ap = ap.rearrange("p (a b) -> p a b", a=shape[1])
        return ap

    def pv(bank, off, shape, dt):
        size = {F32: 4, BF16: 2}[dt]
        n = int(np.prod(shape[1:]))
        ap = ps[bank][0:shape[0], off:off + n * size].bitcast(dt)
        if len(shape) == 3:
            ap = ap.rearrange("p (a b) -> p a b", a=shape[1])
        return ap

    class Alloc:
        def __init__(self, base, limit):
            self.o = base
            self.limit = limit

        def take(self, nbytes):
            o = self.o
            self.o += (nbytes + 63) // 64 * 64
            assert self.o <= self.limit, (self.o, self.limit)
            return o

    KB = 1024
    A = Alloc(0, 40 * KB)
    ident_bf = sb(A.take(256), [128, 128], BF16)
    ident_f = sb(A.take(512), [128, 128], F32)
    iota_p = sb(A.take(4), [128, 1], F32)
    iota_f = sb(A.take(1024), [128, 256], F32)
    mask_c = sb(A.take(512), [128, 128], F32)
    mask_b = sb(A.take(512), [128, 128], F32)
    ublk = sb(A.take(512), [128, 128], F32)
    oblk = sb(A.take(512), [128, 128], F32)
    ustrict = sb(A.take(512), [128, 128], F32)
    ones_f = sb(A.take(512), [128, 128], F32)
    cos2 = sb(A.take(NT * 512), [128, NT, 128], F32)
    sin2 = sb(A.take(NT * 512), [128, NT, 128], F32)
    qdec = sb(A.take(32), [128, 8], F32)
    kdec = sb(A.take(32), [128, 8], F32)
    lb_bc = sb(A.take(4096), [128, 1024], F32)
    oml_bc = sb(A.take(4096), [128, 1024], F32)
    gain_bc = sb(A.take(8192), [128, 2048], F32)
    posb_t = sb(A.take(4), [128, 1], F32)
    negpi = sb(A.take(4), [128, 1], F32)
    eps_t = sb(A.take(4), [128, 1], F32)
    selAB = sb(A.take(8), [128, 2], F32)
    ublk_bf = sb(A.take(256), [128, 128], BF16)
    oblk_bf = sb(A.take(256), [128, 128], BF16)
    selAB_bf = sb(A.take(4), [128, 2], BF16)
    ustrict_bf = sb(A.take(256), [128, 128], BF16)
    ones_bf = sb(A.take(256), [128, 128], BF16)
    B_const = Buf()

    g = lambda f: f

    tmpA = Alloc(40 * KB, 120 * KB)
    t_i = sb(tmpA.take(8192), [128, 2048], F32)
    t_j = sb(tmpA.take(8192), [128, 2048], F32)
    t_k = sb(tmpA.take(8192), [128, 2048], F32)
    B_t = Buf()
    S.op("pool", lambda e: e.iota(iota_f, pattern=[[1, 256]], base=0, channel_multiplier=0,
                                  allow_small_or_imprecise_dtypes=True), writes=[B_const])
    S.op("pool", lambda e: e.iota(iota_p, pattern=[[0, 1]], base=0, channel_multiplier=1,
                                  allow_small_or_imprecise_dtypes=True), writes=[B_const])
    S.op("dve", lambda e: e.memset(ones_f, 1.0), writes=[B_const])
    S.op("dve", lambda e: e.memset(negpi, -math.pi), writes=[B_const])
    S.op("dve", lambda e: e.memset(eps_t, EPS), writes=[B_const])
    S.op("dve", lambda e: e.tensor_scalar(t_i[:, 0:128], iota_f[:, 0:128], iota_p[:, 0:1], None, ALU.subtract),
         reads=[B_const], writes=[B_t])
    S.op("dve", lambda e: e.tensor_single_scalar(mask_c, t_i[:, 0:128], 0.0, ALU.is_ge), reads=[B_t], writes=[B_const])
    S.op("dve", lambda e: e.tensor_single_scalar(ustrict, t_i[:, 0:128], 0.0, ALU.is_gt), reads=[B_t], writes=[B_const])
    S.op("dve", lambda e: e.tensor_single_scalar(ident_f, t_i[:, 0:128], 0.0, ALU.is_equal), reads=[B_t], writes=[B_const])
    S.op("dve", lambda e: e.tensor_copy(ident_bf, ident_f), reads=[B_const], writes=[B_const])
    S.op("dve", lambda e: e.tensor_single_scalar(t_j[:, 0:128], iota_f[:, 0:128], 64.0, ALU.is_ge), reads=[B_const], writes=[B_t])
    S.op("dve", lambda e: e.tensor_single_scalar(t_j[:, 128:129], iota_p[:, 0:1], 64.0, ALU.is_ge), reads=[B_const], writes=[B_t])
    S.op("dve", lambda e: e.tensor_scalar(oblk, t_j[:, 0:128], t_j[:, 128:129], None, ALU.is_equal), reads=[B_t], writes=[B_const])
    S.op("dve", lambda e: e.tensor_copy(selAB[:, 1:2], t_j[:, 128:129]), reads=[B_t], writes=[B_const])
    S.op("dve", lambda e: e.tensor_scalar(selAB[:, 0:1], t_j[:, 128:129], -1.0, 1.0, ALU.mult, ALU.add), reads=[B_t], writes=[B_const])
    S.op("dve", lambda e: e.tensor_tensor(mask_b, mask_c, oblk, ALU.mult), reads=[B_const], writes=[B_const])
    S.op("dve", lambda e: e.tensor_copy(ublk, mask_b), reads=[B_const], writes=[B_const])
    S.op("dve", lambda e: e.tensor_copy(ublk_bf, mask_b), reads=[B_const], writes=[B_const])
    S.op("dve", lambda e: e.tensor_copy(oblk_bf, oblk), reads=[B_const], writes=[B_const])
    S.op("dve", lambda e: e.tensor_copy(selAB_bf, selAB), reads=[B_const], writes=[B_const])
    S.op("dve", lambda e: e.tensor_copy(ustrict_bf, ustrict), reads=[B_const], writes=[B_const])
    S.op("dve", lambda e: e.tensor_copy(ones_bf, ones_f), reads=[B_const], writes=[B_const])
    for h in range(8):
        lg = math.log(1.0 - 2.0 ** (-5.0 - h))
        S.op("act", lambda e, h=h, lg=lg: e.activation(qdec[:, h:h + 1], iota_p[:, 0:1], AF.Exp, scale=lg),
             reads=[B_const], writes=[B_const])
    S.op("dve", lambda e: e.reciprocal(kdec, qdec), reads=[B_const], writes=[B_const])
    for h in range(8):
        lg = math.log(1.0 - 2.0 ** (-5.0 - h))
        S.op("dve", lambda e, h=h, lg=lg: e.tensor_scalar(qdec[:, h:h + 1], qdec[:, h:h + 1], math.exp(lg), None, ALU.mult),
             reads=[B_const], writes=[B_const])
        S.op("dve", lambda e, h=h, lg=lg: e.tensor_scalar(kdec[:, h:h + 1], kdec[:, h:h + 1], math.exp(-lg) * (128.0 ** -0.5), None, ALU.mult),
             reads=[B_const], writes=[B_const])
    bc = lambda ap1d: ap1d.partition_broadcast(128)
    S.op("sp", lambda e: e.dma_start(out=posb_t, in_=posb_d), writes=[B_const], dma=True)
    S.op("sp", lambda e: e.dma_start(out=gain_bc[:, 0:1024], in_=bc(rgain_d)), pwrites=[B_const], dma=True)
    S.op("sp", lambda e: e.dma_start(out=gain_bc[:, 1024:2048], in_=bc(hgain_d)), pwrites=[B_const], dma=True)
    S.op("sp", lambda e: e.dma_start(out=t_i[:, 0:1024], in_=bc(lbl_d[0])), writes=[B_t], dma=True)
    S.op("sp", lambda e: e.dma_start(out=t_j[:, 0:1024], in_=bc(lbl_d[1])), pwrites=[B_t], dma=True)
    S.op("dve", lambda e: e.tensor_tensor(t_k[:, 0:1024], t_i[:, 0:1024], t_j[:, 0:1024], ALU.subtract), reads=[B_t], pwrites=[B_t])
    S.op("act", lambda e: e.activation(lb_bc, t_k[:, 0:1024], AF.Sigmoid), reads=[B_t], pwrites=[B_const])
    S.op("dve", lambda e: e.tensor_scalar(oml_bc, lb_bc, -1.0, 1.0, ALU.mult, ALU.add), reads=[B_const], pwrites=[B_const])
    invf = t_i[:, 1024:1088]
    S.op("act", lambda e: e.activation(invf, iota_f[:, 0:64], AF.Exp, scale=-math.log(10000.0) / 64.0),
         reads=[B_const, B_t], pwrites=[B_t])
    TWO_PI = 2.0 * math.pi
    ni_t = sb(40 * KB + 3 * 8192, [128, 128], I32)
    for t in range(NT):
        pc = t_j[:, 1100 + t:1101 + t]
        ang = t_k[:, 1024:1152]
        uu = t_k[:, 1152:1280]
        nf = t_k[:, 1280:1408]
        rr = t_k[:, 1408:1536]
        mm = t_k[:, 1536:1664]
        sc_ = t_k[:, 1664:1792]
        S.op("dve", lambda e, pc=pc, t=t: e.tensor_scalar(pc, iota_p[:, 0:1], posb_t[:, 0:1], float(128 * t), ALU.add, ALU.add),
             reads=[B_const, B_t], writes=[B_t])
        S.op("dve", lambda e, pc=pc: e.tensor_single_scalar(pc, pc, 0.0, ALU.max), reads=[B_t], writes=[B_t])
        S.op("dve", lambda e, pc=pc: e.tensor_scalar(ang[:, 0:64], invf, pc, None, ALU.mult), reads=[B_t], writes=[B_t])
        S.op("dve", lambda e: e.tensor_scalar(ang[:, 64:128], ang[:, 0:64], math.pi / 2.0, None, ALU.add), reads=[B_t], writes=[B_t])
        S.op("dve", lambda e: e.tensor_scalar(uu, ang, 1.0 / TWO_PI, None, ALU.mult), reads=[B_t], writes=[B_t])
        S.op("dve", lambda e: e.tensor_copy(ni_t, uu), reads=[B_t], writes=[B_t])
        S.op("dve", lambda e: e.tensor_copy(nf, ni_t), reads=[B_t], writes=[B_t])
        S.op("dve", lambda e: e.scalar_tensor_tensor(rr, nf, -TWO_PI, ang, ALU.mult, ALU.add), reads=[B_t], writes=[B_t])
        S.op("dve", lambda e: e.tensor_single_scalar(mm, rr, math.pi, ALU.is_gt), reads=[B_t], writes=[B_t])
        S.op("dve", lambda e: e.scalar_tensor_tensor(rr, mm, -TWO_PI, rr, ALU.mult, ALU.add), reads=[B_t], writes=[B_t])
        S.op("dve", lambda e: e.tensor_single_scalar(mm, rr, -math.pi, ALU.is_lt), reads=[B_t], writes=[B_t])
        S.op("dve", lambda e: e.scalar_tensor_tensor(rr, mm, TWO_PI, rr, ALU.mult, ALU.add), reads=[B_t], writes=[B_t])
        S.op("act", lambda e: e.activation(sc_, rr, AF.Sin), reads=[B_t], writes=[B_t])
        S.op("dve", lambda e, t=t: e.tensor_scalar(sin2[:, t, 0:64], sc_[:, 0:64], -1.0, None, ALU.mult), reads=[B_t], pwrites=[B_const])
        S.op("dve", lambda e, t=t: e.tensor_copy(sin2[:, t, 64:128], sc_[:, 0:64]), reads=[B_t], pwrites=[B_const])
        S.op("dve", lambda e, t=t: e.tensor_copy(cos2[:, t, 0:64], sc_[:, 64:128]), reads=[B_t], pwrites=[B_const])
        S.op("dve", lambda e, t=t: e.tensor_copy(cos2[:, t, 64:128], sc_[:, 64:128]), reads=[B_t], pwrites=[B_const])

    if stop_after == "const":
        dh = dbg["h"]
        S.op("sp", lambda e: e.dma_start(out=dh[0:128, 0:2048], in_=cos2.rearrange("p a b -> p (a b)")), reads=[B_const], dma=True)
        S.op("sp", lambda e: e.dma_start(out=dh[128:256, 0:2048], in_=sin2.rearrange("p a b -> p (a b)")), reads=[B_const], dma=True)
        S.op("sp", lambda e: e.dma_start(out=dh[256:384, 0:1024], in_=lb_bc), reads=[B_const], dma=True)
        S.op("sp", lambda e: e.dma_start(out=dh[384:512, 0:2048], in_=gain_bc), reads=[B_const], dma=True)
        S.op("sp", lambda e: e.dma_start(out=dh[512:640, 0:128], in_=mask_b), reads=[B_const], dma=True)
        S.op("sp", lambda e: e.dma_start(out=dh[640:768, 0:128], in_=ident_f), reads=[B_const], dma=True)
        S.op("sp", lambda e: e.dma_start(out=dh[768:896, 0:8], in_=qdec), reads=[B_const], dma=True)
        S.op("sp", lambda e: e.dma_start(out=dh[896:1024, 0:8], in_=kdec), reads=[B_const], dma=True)
        return finish(nc, S, es, esem, dsem)
    M0 = 40 * KB
    xT_bf = sb(M0, [128, 16, NT * 128], BF16)
    win_bf = [sb(M0 + 64 * KB + i * 16 * KB, [128, 16, 512], BF16) for i in range(2)]
    mixT = sb(M0 + 96 * KB, [128, 16, NOWN * 128], BF16)
    WK = Alloc(M0 + 128 * KB, 200 * KB)
    B_xT = Buf(); B_win = [Buf(), Buf()]; B_mixT = Buf()
    for k in range(16):
        S.op("pool", lambda e, k=k: e.dma_start(out=xT_bf[:, k, :], in_=xT_d[k * 128:(k + 1) * 128, :]),
             after=[B_t, B_const], pwrites=[B_xT], dma=True)

    def wtile(shape, dt):
        size = {F32: 4, BF16: 2, I32: 4, U32: 4}[dt]
        return sb(WK.take(int(np.prod(shape[1:])) * size), shape, dt), Buf()

    NSET = 2
    W = []
    for s_ in range(NSET):
        d_ = {}
        for nm in ("qa", "qb", "ka", "kb", "sg", "on", "gs", "sig", "ff", "logf", "kk", "eb", "enb", "eblb", "ekb"):
            d_[nm] = wtile([128, 128], F32)
        for nm in ("lhi", "llo", "qd", "kdi", "kd2", "kd2A", "kd2B", "v", "qdT", "qdTA", "qdTB", "kdiT", "pT", "og"):
            d_[nm] = wtile([128, 128], BF16)
        d_["psb"] = wtile([128, 512], F32)
        d_["st6"] = wtile([128, 6], F32)
        d_["mv"] = wtile([128, 4], F32)
        d_["ebT"] = wtile([128, 4], F32)
        W.append(d_)
    ST = [(wtile([128, 128], F32), wtile([128, 128], BF16), wtile([128, 128], BF16)) for _ in range(2)]
    for d_ in W:
        for nm in ("qdTA", "qdTB"):
            S.op("dve", lambda e, t_=d_[nm][0]: e.memset(t_, 0.0), after=[B_t, B_const], writes=[d_[nm][1]])

    P_ps = [pv(0, 0, [128, 512], F32), pv(1, 0, [128, 512], F32)]
    B_P = [Buf(), Buf()]
    Bk = [Buf() for _ in range(8)]
    b_ps = pv(2, 0, [128, 128], F32); B_bps = Bk[2]
    blb_ps = pv(2, 512, [128, 128], F32); B_blbps = Bk[2]
    blT_ps = pv(2, 1024, [128, 4], F32); B_blTps = Bk[2]
    TPS = [(pv(bk_, 0, [128, 128], BF16), pv(bk_, 256, [128, 128], BF16), pv(bk_, 512, [128, 128], BF16), Bk[bk_]) for bk_ in (3, 7)]
    sc_ps = pv(4, 0, [128, 128], F32); B_scps = Bk[4]
    o_ps = pv(5, 0, [128, 128], F32); B_ops = Bk[5]
    dSa_ps = pv(6, 0, [128, 128], F32); B_dSa = Bk[6]
    dSb_ps = pv(6, 512, [128, 128], F32); B_dSb = Bk[6]

    if debug:
        S.op("dve", lambda e: e.memset(mixT.rearrange("p a b -> p (a b)"), 0.0), after=[B_t, B_const], writes=[B_mixT])
    steps = [(hb, t, 0) for hb in range(16) for t in range(NT)]

    def load_win(hb, bi):
        for q in range(4):
            S.op("pool", lambda e, hb=hb, q=q, bi=bi: e.dma_start(
                out=win_bf[bi][:, 4 * q:4 * q + 4, :],
                in_=win_d[hb, 512 * q:512 * (q + 1), :].rearrange("(k p) f -> p k f", p=128)),
                writes=[B_win[bi]] if q == 0 else [], pwrites=[] if q == 0 else [B_win[bi]], dma=True)

    def proj(i):
        hb, t, sl = steps[i]
        pi = i % 2
        c0, c1 = (0, 512) if t >= NT - NOWN else (128, 384)
        for k in range(16):
            S.op("pe", lambda e, hb=hb, t=t, k=k, pi=pi, sl=sl, c0=c0, c1=c1: e.matmul(
                P_ps[pi][:, c0:c1], lhsT=xT_bf[:, k, t * 128:(t + 1) * 128], rhs=win_bf[sl][:, k, c0:c1],
                start=(k == 0), stop=(k == 15)),
                reads=[B_xT, B_win[sl]], writes=[B_P[pi]] if k == 0 else [], pwrites=[] if k == 0 else [B_P[pi]])

    def rotary(P, off, t, a, Ba, b, Bb, BP):
        S.op("dve", lambda e: e.tensor_tensor(a, P[:, off:off + 128], cos2[:, t, :], ALU.mult), reads=[BP, B_const], writes=[Ba])
        S.op("dve", lambda e: e.tensor_tensor(b[:, 0:64], P[:, off + 64:off + 128], sin2[:, t, 0:64], ALU.mult), reads=[BP, B_const], writes=[Bb])
        S.op("dve", lambda e: e.tensor_tensor(b[:, 64:128], P[:, off:off + 64], sin2[:, t, 64:128], ALU.mult), reads=[BP, B_const], pwrites=[Bb])
        S.op("dve", lambda e: e.tensor_tensor(a, a, b, ALU.add), reads=[Bb], writes=[Ba])

    def transpose_to(src, Bsrc, pst, Bpst, dst, Bdst, dst_p=False):
        S.op("pe", lambda e: e.transpose(pst, src, ident_bf), reads=[Bsrc, B_const], writes=[Bpst])
        S.op("act", lambda e: e.copy(dst, pst), reads=[Bpst], writes=[] if dst_p else [Bdst], pwrites=[Bdst] if dst_p else [])

    def head_out(w, hb, t, own_i, with_mean, ogT_ps, B_ogTps):
        on, Bon = w["on"]; gs, Bgs = w["gs"]; sg, Bsg = w["sg"]; og, Bog = w["og"]
        st6, Bst = w["st6"]; mv, Bmv = w["mv"]
        S.op("dve", lambda e: e.bn_stats(st6, o_ps), reads=[B_ops], writes=[Bst])
        S.op("dve", lambda e: e.bn_aggr(mv[:, 0:2], st6), reads=[Bst], writes=[Bmv])
        if with_mean:
            S.op("act", lambda e: e.activation(mv[:, 3:4], mv[:, 1:2], AF.Ln, bias=eps_t[:, 0:1]), reads=[Bmv, B_const], writes=[Bmv])
            S.op("act", lambda e: e.activation(mv[:, 2:3], mv[:, 3:4], AF.Exp, scale=-0.5), reads=[Bmv], writes=[Bmv])
            S.op("dve", lambda e: e.tensor_scalar(on, o_ps, mv[:, 0:1], mv[:, 2:3], ALU.subtract, ALU.mult), reads=[B_ops, Bmv], writes=[Bon])
        else:
            S.op("dve", lambda e: e.scalar_tensor_tensor(mv[:, 3:4], mv[:, 0:1], mv[:, 0:1], mv[:, 1:2], ALU.mult, ALU.add), reads=[Bmv], writes=[Bmv])
            S.op("act", lambda e: e.activation(mv[:, 3:4], mv[:, 3:4], AF.Ln, bias=eps_t[:, 0:1]), reads=[Bmv, B_const], writes=[Bmv])
            S.op("act", lambda e: e.activation(mv[:, 2:3], mv[:, 3:4], AF.Exp, scale=-0.5), reads=[Bmv], writes=[Bmv])
            S.op("dve", lambda e: e.tensor_scalar(on, o_ps, mv[:, 2:3], None, ALU.mult), reads=[B_ops, Bmv], writes=[Bon])
        S.op("dve", lambda e: e.tensor_tensor(gs, sg, gain_bc[:, hb * 128:(hb + 1) * 128], ALU.mult), reads=[Bsg, B_const], writes=[Bgs])
        S.op("dve", lambda e: e.tensor_tensor(og, on, gs, ALU.mult), reads=[Bon, Bgs], writes=[Bog])
        transpose_to(og, Bog, ogT_ps, B_ogTps, mixT[:, hb, own_i * 128:(own_i + 1) * 128], B_mixT, dst_p=True)

    import os as _os
    LVL = float(_os.environ.get('REC_LEVEL', '9'))

    def rec(i):
        hb, t, sl = steps[i]
        pi = i % 2
        w = W[i % NSET]
        P, BP = w["psb"]
        own = t >= NT - NOWN
        (T_f, B_T), (SbA, B_SbA), (SbB, B_SbB) = ST[sl]
        qdT_ps, kdiT_ps, ogT_ps, B_tp = TPS[sl]
        B_qdTps = B_kdiTps = B_ogTps = B_tp
        if own:
            S.op("act", lambda e: e.copy(P, P_ps[pi]), reads=[B_P[pi]], writes=[BP])
        else:
            S.op("act", lambda e: e.copy(P[:, 128:384], P_ps[pi][:, 128:384]), reads=[B_P[pi]], writes=[BP])
        if t == 0 or len(steps) == 1:
            S.op("dve", lambda e: e.memset(T_f, 0.0), writes=[B_T])
            S.op("dve", lambda e: e.memset(SbA, 0.0), writes=[B_SbA])
        v, Bv = w["v"]; kdi, Bkdi = w["kdi"]; qd, Bqd = w["qd"]
        qdT, BqdT = w["qdT"]; kdiT, BkdiT = w["kdiT"]; pT, BpT = w["pT"]; sg, Bsg = w["sg"]
        S.op("act", lambda e: e.copy(v, P[:, 256:384]), reads=[BP], writes=[Bv])
        if own and LVL >= 0.2:
            S.op("act", lambda e: e.activation(sg, P[:, 384:512], AF.Exp, scale=-1.0), reads=[BP], writes=[Bsg])
            S.op("act", lambda e: e.activation(sg, sg, AF.Ln, bias=ones_f[:, 0:1]), reads=[B_const], writes=[Bsg])
            S.op("act", lambda e: e.activation(sg, sg, AF.Exp, scale=-1.0), writes=[Bsg])
            S.op("dve", lambda e: e.tensor_tensor(sg, sg, P[:, 384:512], ALU.mult), reads=[BP], writes=[Bsg])
        if hb < 8:
            h = hb
            g128 = (1.0 - 2.0 ** (-5.0 - h)) ** 128
            ka, Bka = w["ka"]; kb, Bkb = w["kb"]; qa, Bqa = w["qa"]; qb, Bqb = w["qb"]
            if LVL >= 0.4:
                rotary(P, 128, t, ka, Bka, kb, Bkb, BP)
            if LVL >= 0.6:
                S.op("act", lambda e: e.activation(kdi, ka, AF.Copy, scale=kdec[:, h:h + 1]), reads=[Bka, B_const], writes=[Bkdi])
            if own and LVL >= 2:
                rotary(P, 0, t, qa, Bqa, qb, Bqb, BP)
                S.op("act", lambda e: e.activation(qd, qa, AF.Copy, scale=qdec[:, h:h + 1]), reads=[Bqa, B_const], writes=[Bqd])
                transpose_to(qd, Bqd, qdT_ps, B_qdTps, qdT, BqdT)
                transpose_to(kdi, Bkdi, kdiT_ps, B_kdiTps, kdiT, BkdiT)
            if own and LVL >= 3:
                S.op("pe", lambda e: e.matmul(sc_ps, lhsT=kdiT, rhs=qdT, start=True, stop=True), reads=[BkdiT, BqdT], writes=[B_scps])
                S.op("dve", lambda e: e.tensor_tensor(pT, sc_ps, mask_c, ALU.mult), reads=[B_scps, B_const], writes=[BpT])
                S.op("pe", lambda e: e.matmul(o_ps, lhsT=pT, rhs=v, start=True, stop=False), reads=[BpT, Bv], writes=[B_ops])
                S.op("pe", lambda e: e.matmul(o_ps, lhsT=qdT, rhs=SbA, start=False, stop=True), reads=[BqdT, B_SbA], pwrites=[B_ops])
            if LVL >= 4:
                S.op("pe", lambda e: e.matmul(dSa_ps, lhsT=kdi, rhs=v, start=True, stop=True), reads=[Bkdi, Bv], writes=[B_dSa])
                S.op("dve", lambda e: e.tensor_tensor(T_f, T_f, dSa_ps, ALU.add), reads=[B_dSa], writes=[B_T])
                S.op("act", lambda e: e.activation(T_f, T_f, AF.Copy, scale=g128), writes=[B_T])
                S.op("act", lambda e: e.copy(SbA, T_f), reads=[B_T], writes=[B_SbA])
            if own and LVL >= 5:
                head_out(w, hb, t, t - (NT - NOWN), True, ogT_ps, B_ogTps)
        else:
            h = hb - 8
            sig, Bsig = w["sig"]; ff, Bff = w["ff"]; logf, Blogf = w["logf"]; kk, Bkk = w["kk"]
            eb, Beb = w["eb"]; enb, Benb = w["enb"]; eblb, Beblb = w["eblb"]; ekb, Bekb = w["ekb"]
            kd2, Bkd2 = w["kd2"]; ebT, BebT = w["ebT"]
            if LVL >= 0.2:
                S.op("act", lambda e: e.activation(sig, P[:, 128:256], AF.Exp, scale=-1.0), reads=[BP], writes=[Bsig])
                S.op("act", lambda e: e.activation(sig, sig, AF.Ln, bias=ones_f[:, 0:1]), reads=[B_const], writes=[Bsig])
                S.op("act", lambda e: e.activation(sig, sig, AF.Exp, scale=-1.0), writes=[Bsig])
            if LVL >= 0.2:
                S.op("dve", lambda e: e.tensor_tensor(ff, sig, oml_bc[:, h * 128:(h + 1) * 128], ALU.mult), reads=[Bsig, B_const], writes=[Bff])
            if LVL >= 0.2:
                S.op("dve", lambda e: e.tensor_tensor(ff, ff, lb_bc[:, h * 128:(h + 1) * 128], ALU.add), reads=[B_const], writes=[Bff])
            if LVL >= 0.2:
                S.op("act", lambda e: e.activation(logf, ff, AF.Ln), reads=[Bff], writes=[Blogf])
            if LVL >= 0.2:
                S.op("dve", lambda e: e.tensor_scalar(kk, ff, -1.0, 1.0, ALU.mult, ALU.add), reads=[Bff], writes=[Bkk])
            lhi, Blhi = w["lhi"]; llo, Bllo = w["llo"]
            if LVL >= 0.4:
                S.op("act", lambda e: e.copy(lhi, logf), reads=[Blogf], writes=[Blhi])
                S.op("act", lambda e: e.copy(sig, lhi), reads=[Blhi], writes=[Bsig])
                S.op("dve", lambda e: e.tensor_tensor(llo, logf, sig, ALU.subtract), reads=[Blogf, Bsig], writes=[Bllo])
                S.op("pe", lambda e: e.matmul(b_ps, lhsT=ublk_bf, rhs=lhi, start=True, stop=False), reads=[Blhi, B_const], writes=[B_bps])
                S.op("pe", lambda e: e.matmul(b_ps, lhsT=ublk_bf, rhs=llo, start=False, stop=True), reads=[Bllo, B_const], pwrites=[B_bps])
                S.op("pe", lambda e: e.matmul(blb_ps, lhsT=oblk_bf, rhs=lhi, start=True, stop=False), reads=[Blhi, B_const], pwrites=[B_blbps])
                S.op("pe", lambda e: e.matmul(blb_ps, lhsT=oblk_bf, rhs=llo, start=False, stop=True), reads=[Bllo, B_const], pwrites=[B_blbps])
            if LVL >= 0.6:
                S.op("pe", lambda e: e.matmul(blT_ps[:, 0:2], lhsT=lhi, rhs=selAB_bf, start=True, stop=False), reads=[Blhi, B_const], pwrites=[B_blTps])
                S.op("pe", lambda e: e.matmul(blT_ps[:, 0:2], lhsT=llo, rhs=selAB_bf, start=False, stop=True), reads=[Bllo, B_const], pwrites=[B_blTps])
            if LVL >= 0.4:
                S.op("act", lambda e: e.activation(enb, b_ps, AF.Exp, scale=-1.0), reads=[B_bps], writes=[Benb])
            if LVL >= 0.4:
                S.op("act", lambda e: e.activation(eblb, blb_ps, AF.Exp), reads=[B_blbps], writes=[Beblb])
            if LVL >= 0.6:
                S.op("act", lambda e: e.activation(ebT[:, 0:2], blT_ps[:, 0:2], AF.Exp), reads=[B_blTps], writes=[BebT])
            if LVL >= 0.8:
                S.op("dve", lambda e: e.tensor_tensor(ekb, eblb, enb, ALU.mult), reads=[Beblb, Benb], writes=[Bekb])
            if LVL >= 0.8:
                S.op("dve", lambda e: e.tensor_tensor(kd2, kk, ekb, ALU.mult), reads=[Bkk, Bekb], writes=[Bkd2])
            kd2A, Bkd2A = w["kd2A"]; kd2B, Bkd2B = w["kd2B"]; qdTA, BqdTA = w["qdTA"]; qdTB, BqdTB = w["qdTB"]
            if LVL >= 0.8:
                S.op("dve", lambda e: e.tensor_scalar(kd2A, kd2, selAB[:, 0:1], None, ALU.mult), reads=[Bkd2, B_const], writes=[Bkd2A])
            if LVL >= 0.8:
                S.op("dve", lambda e: e.tensor_scalar(kd2B, kd2, selAB[:, 1:2], None, ALU.mult), reads=[Bkd2, B_const], writes=[Bkd2B])
            if own and LVL >= 2:
                S.op("act", lambda e: e.activation(eb, b_ps, AF.Exp), reads=[B_bps], writes=[Beb])
                S.op("dve", lambda e: e.tensor_tensor(qd, P[:, 0:128], eb, ALU.mult), reads=[BP, Beb], writes=[Bqd])
                S.op("dve", lambda e: e.tensor_tensor(kdi, kk, enb, ALU.mult), reads=[Bkk, Benb], writes=[Bkdi])
                transpose_to(qd, Bqd, qdT_ps, B_qdTps, qdT, BqdT)
                S.op("act", lambda e: e.copy(qdTA[:, 0:64], qdT_ps[:, 0:64]), reads=[B_qdTps], pwrites=[BqdTA])
                S.op("act", lambda e: e.copy(qdTB[:, 64:128], qdT_ps[:, 64:128]), reads=[B_qdTps], pwrites=[BqdTB])
                transpose_to(kdi, Bkdi, kdiT_ps, B_kdiTps, kdiT, BkdiT)
                S.op("pe", lambda e: e.matmul(sc_ps, lhsT=kdiT, rhs=qdT, start=True, stop=True), reads=[BkdiT, BqdT], writes=[B_scps])
                S.op("dve", lambda e: e.tensor_tensor(pT, sc_ps, mask_b, ALU.mult), reads=[B_scps, B_const], writes=[BpT])
            if own and LVL >= 3:
                S.op("pe", lambda e: e.matmul(o_ps, lhsT=pT, rhs=v, start=True, stop=False), reads=[BpT, Bv], writes=[B_ops])
                S.op("pe", lambda e: e.matmul(o_ps, lhsT=qdTA, rhs=SbA, start=False, stop=False), reads=[BqdTA, B_SbA], pwrites=[B_ops])
            if LVL < 4:
                return
            S.op("pe", lambda e: e.matmul(dSa_ps, lhsT=kd2A, rhs=v, start=True, stop=True), reads=[Bkd2A, Bv], writes=[B_dSa])
            S.op("dve", lambda e: e.scalar_tensor_tensor(T_f, T_f, ebT[:, 0:1], dSa_ps, ALU.mult, ALU.add), reads=[B_dSa, BebT], writes=[B_T])
            S.op("act", lambda e: e.copy(SbB, T_f), reads=[B_T], writes=[B_SbB])
            if LVL < 5:
                return
            if own:
                S.op("pe", lambda e: e.matmul(o_ps, lhsT=qdTB, rhs=SbB, start=False, stop=True), reads=[BqdTB, B_SbB], pwrites=[B_ops])
            S.op("pe", lambda e: e.matmul(dSb_ps, lhsT=kd2B, rhs=v, start=True, stop=True), reads=[Bkd2B, Bv], writes=[B_dSb])
            S.op("dve", lambda e: e.scalar_tensor_tensor(T_f, T_f, ebT[:, 1:2], dSb_ps, ALU.mult, ALU.add), reads=[B_dSb, BebT], writes=[B_T])
            S.op("act", lambda e: e.copy(SbA, T_f), reads=[B_T], writes=[B_SbA])
            if own and LVL >= 6:
                head_out(w, hb, t, t - (NT - NOWN), False, ogT_ps, B_ogTps)

    if stop_after == "proj":
        steps[:] = [(hb, t, 0) for hb in debug_heads for t in range(NT)]
        load_win(steps[0][0], 0); proj(0)
        tmp = sb(WK.take(2048), [128, 512], F32); Btmp = Buf()
        S.op("act", lambda e: e.copy(tmp, P_ps[0]), reads=[B_P[0]], writes=[Btmp])
        S.op("sp", lambda e: e.dma_start(out=dbg["h"][0:128, 0:512], in_=tmp), reads=[Btmp], dma=True)
        return finish(nc, S, es, esem, dsem)
    if stop_after == "rec1":
        steps[:] = [(debug_heads[0], 8, 0)]
        load_win(steps[0][0], 0); proj(0); rec(0)
        w0 = W[0]
        dh = dbg["h"]
        tmp = sb(WK.take(2048), [128, 512], F32); Btmp = Buf()
        S.op("dve", lambda e: e.tensor_copy(tmp[:, 0:128], w0["kdi"][0]), reads=[w0["kdi"][1]], writes=[Btmp])
        S.op("dve", lambda e: e.tensor_copy(tmp[:, 128:256], w0["v"][0]), reads=[w0["v"][1]], pwrites=[Btmp])
        S.op("dve", lambda e: e.tensor_copy(tmp[:, 256:384], w0["qdT"][0]), reads=[w0["qdT"][1]], pwrites=[Btmp])
        S.op("dve", lambda e: e.tensor_copy(tmp[:, 384:512], w0["pT"][0]), reads=[w0["pT"][1]], pwrites=[Btmp])
        S.op("sp", lambda e: e.dma_start(out=dh[0:128, 0:512], in_=tmp), reads=[Btmp], dma=True)
        S.op("sp", lambda e: e.dma_start(out=dh[128:256, 0:128], in_=ST[0][0][0]), reads=[ST[0][0][1]], dma=True)
        S.op("sp", lambda e: e.dma_start(out=dh[256:384, 0:128], in_=w0["on"][0]), reads=[w0["on"][1]], dma=True)
        return finish(nc, S, es, esem, dsem)
    heads = list(range(16)) if not debug else list(debug_heads)
    steps[:] = []
    for pi_ in range(0, len(heads), 2):
        pair = heads[pi_:pi_ + 2]
        for t in range(NT):
            for sl, hb in enumerate(pair):
                steps.append((hb, t, sl))
    loaded = set()
    def ensure_win(i):
        hb, _, sl = steps[i]
        if hb not in loaded:
            load_win(hb, sl); loaded.add(hb)
    ensure_win(0); proj(0)
    for i in range(len(steps)):
        if i + 1 < len(steps):
            ensure_win(i + 1); proj(i + 1)
        rec(i)
    if stop_after == "mixer":
        S.op("sp", lambda e: e.dma_start(out=dbg["mixT"], in_=mixT.rearrange("p a b -> p (a b)")), reads=[B_mixT], dma=True)
        return finish(nc, S, es, esem, dsem)

    B_M = [B_xT, B_win[0], B_win[1], B_t] + [b for st_ in ST for (_, b) in st_] + [b for d_ in W for (_, b) in d_.values()]
    R0 = 40 * KB
    NRING = 8
    ring = [sb(R0 + i * 4096, [128, 2048], BF16) for i in range(NRING)]
    B_ring = [Buf() for _ in range(NRING)]
    xo = sb(R0 + 32 * KB, [128, 2048], F32); B_xo = Buf()
    ht = [sb(R0 + 40 * KB + i * 8192, [128, 2048], F32) for i in range(2)]; B_ht = [Buf(), Buf()]
    hbf = sb(R0 + 56 * KB, [128, 2048], BF16); B_hbf = Buf()
    hT = sb(R0 + 60 * KB, [128, 16, 128], BF16); B_hT = Buf()
    ln_g = sb(R0 + 64 * KB, [128, 2048], F32); ln_b = sb(R0 + 72 * KB, [128, 2048], F32); B_ln = Buf()
    wr_bf = sb(R0 + 80 * KB, [128, 16, 256], BF16); B_wr = Buf()
    em_bf = sb(R0 + 88 * KB, [128, 8, 256], BF16); B_em = Buf()
    Q = Alloc(168 * KB, 200 * KB)
    rb_bc = sb(Q.take(1024), [128, 256], F32); B_rb = Buf()
    slot_all = sb(Q.take(256), [128, 8, 8], I32); B_slot = Buf()
    wk_all = sb(Q.take(256), [128, 8, 8], F32); B_wk = Buf()
    rt = {}
    for nm in ("sc", "sel", "selm", "em", "gd", "csb"):
        rt[nm] = (sb(Q.take(1024), [128, 256], F32), Buf())
    gm = sb(Q.take(256), [128, 8, 8], F32); gsum = sb(Q.take(32), [128, 8], F32); m2 = sb(Q.take(32), [128, 8], F32)
    gmask = sb(Q.take(32), [128, 8], F32); pen = sb(Q.take(32), [128, 8], F32); m8 = sb(Q.take(32), [128, 8], F32)
    mk = sb(Q.take(32), [128, 8], F32); idxu = sb(Q.take(32), [128, 8], U32); idxf = sb(Q.take(32), [128, 8], F32)
    posf = sb(Q.take(32), [128, 8], F32); slotf = sb(Q.take(32), [128, 8], F32); okf = sb(Q.take(32), [128, 8], F32)
    ssum = sb(Q.take(8), [128, 2], F32); st24 = sb(Q.take(96), [128, 4, 6], F32); mvl = sb(Q.take(16), [128, 4], F32)
    B_r = Buf()
    a_f = sb(Q.take(2048), [128, 512], F32); B_af = Buf()
    a_bf = sb(Q.take(1024), [128, 512], BF16); B_abf = Buf()
    aT = sb(Q.take(1024), [128, 4, 128], BF16); B_aT = Buf()
    ysb = sb(Q.take(8192), [128, 2048], F32); B_ysb = Buf()
    xe = sb(Q.take(4096), [128, 2048], BF16); B_xe = Buf()
    xeT = sb(Q.take(2048), [128, 16, 64], BF16); B_xeT = Buf()
    zer = sb(Q.take(4096), [128, 2048], BF16); B_zer = Buf()

    B_WS = [B_xo, B_ht[0], B_ht[1], B_hbf, B_hT, B_ln, B_wr, B_em]
    Pb = [pv(i, 0, [128, 512], F32) for i in range(8)]
    Pbf = [pv(i, 0, [128, 1024], BF16) for i in range(8)]

    S.op("dve", lambda e: e.memset(zer, 0.0), after=B_M, writes=[B_zer])
    B_xbuf = Buf(); B_ybuf = Buf(); B_accb = Buf()
    nz = NSLOT // 128
    for i in range(0, ne * CAP // 128):
        S.op("sp", lambda e, i=i: e.dma_start(out=xbuf_d[i * 128:(i + 1) * 128, :], in_=zer), reads=[B_zer], pwrites=[B_xbuf], dma=True)
    S.op("dve", lambda e: e.memset(ysb, 0.0), after=B_M, writes=[B_ysb])
    S.op("pool", lambda e: e.dma_start(out=wr_bf[:, 0:8, :], in_=wr_d[0:1024, :].rearrange("(k p) f -> p k f", p=128)), after=B_M, writes=[B_wr], dma=True)
    S.op("pool", lambda e: e.dma_start(out=wr_bf[:, 8:16, :], in_=wr_d[1024:2048, :].rearrange("(k p) f -> p k f", p=128)), pwrites=[B_wr], dma=True)
    S.op("sp", lambda e: e.dma_start(out=rb_bc, in_=bc(rb_d)), after=B_M, writes=[B_rb], dma=True)
    S.op("sp", lambda e: e.dma_start(out=ln_g, in_=bc(ln_d[0])), after=B_M, writes=[B_ln], dma=True)
    S.op("sp", lambda e: e.dma_start(out=ln_b, in_=bc(ln_d[1])), pwrites=[B_ln], dma=True)

    piece_ctr = [0]
    ring_active = [NRING]
    NEXTRA = 16
    for i in range(NEXTRA):
        ring.append(sb(R0 + 32 * KB + i * 4096, [128, 2048], BF16))
        B_ring.append(Buf())
    first_use = set()

    def load_piece(src_ap, three_d):
        i = piece_ctr[0] % ring_active[0]
        piece_ctr[0] += 1
        dst = ring[i].rearrange("p (a b) -> p a b", a=4) if three_d else ring[i]
        aft = ()
        if i not in first_use:
            first_use.add(i)
            aft = B_M if i < NRING else B_WS
        S.op("pool", lambda e: e.dma_start(out=dst, in_=src_ap), after=aft, writes=[B_ring[i]], dma=True)
        return i

    def layer_norm(src, Bsrc, g_ap, b_ap):
        for q in range(4):
            S.op("dve", lambda e, q=q: e.bn_stats(st24[:, q, :], src[:, q * 512:(q + 1) * 512]), reads=[Bsrc], writes=[B_r] if q == 0 else [], pwrites=[] if q == 0 else [B_r])
        S.op("dve", lambda e: e.bn_aggr(mvl[:, 0:2], st24.rearrange("p a b -> p (a b)")), reads=[B_r], pwrites=[B_r])
        S.op("act", lambda e: e.activation(mvl[:, 3:4], mvl[:, 1:2], AF.Ln, bias=eps_t[:, 0:1]), reads=[B_r, B_const], pwrites=[B_r])
        S.op("act", lambda e: e.activation(mvl[:, 2:3], mvl[:, 3:4], AF.Exp, scale=-0.5), reads=[B_r], pwrites=[B_r])
        S.op("dve", lambda e: e.tensor_scalar(src, src, mvl[:, 0:1], mvl[:, 2:3], ALU.subtract, ALU.mult), reads=[B_r], writes=[Bsrc])
        S.op("dve", lambda e: e.tensor_tensor(src, src, g_ap, ALU.mult), reads=[B_ln], writes=[Bsrc])
        S.op("dve", lambda e: e.tensor_tensor(src, src, b_ap, ALU.add), reads=[B_ln], writes=[Bsrc])

    def ffn(xt_chunk, Bxt, M, g_ap, u_ap, d_ap):
        for which, w_ap, bank in (("g", g_ap, 0), ("u", u_ap, 1)):
            for q in range(4):
                ri = load_piece(w_ap[512 * q:512 * (q + 1), :].rearrange("(k p) f -> p k f", p=128), True)
                rv = ring[ri].rearrange("p (a b) -> p a b", a=4)
                for kk in range(4):
                    k = 4 * q + kk
                    S.op("pe", lambda e, rv=rv, kk=kk, k=k, bank=bank: e.matmul(Pb[bank][0:M, :], lhsT=xt_chunk(k), rhs=rv[:, kk, :], start=(k == 0), stop=(k == 15)),
                         reads=[Bxt, B_ring[ri]], writes=[Bk[bank]] if k == 0 else [], pwrites=[] if k == 0 else [Bk[bank]])
        S.op("act", lambda e: e.activation(a_f[0:M, :], Pb[0][0:M, :], AF.Silu), reads=[Bk[0]], writes=[B_af])
        S.op("dve", lambda e: e.tensor_tensor(a_bf[0:M, :], a_f[0:M, :], Pb[1][0:M, :], ALU.mult), reads=[B_af, Bk[1]], writes=[B_abf])
        for c in range(4):
            S.op("pe", lambda e, c=c: e.transpose(Pbf[2][:, c * 128:c * 128 + M], a_bf[0:M, c * 128:(c + 1) * 128], ident_bf[0:M, 0:M]),
                 reads=[B_abf, B_const], writes=[Bk[2]] if c == 0 else [], pwrites=[] if c == 0 else [Bk[2]])
        for c in range(4):
            S.op("act", lambda e, c=c: e.copy(aT[:, c, 0:M], Pbf[2][:, c * 128:c * 128 + M]), reads=[Bk[2]], writes=[B_aT] if c == 0 else [], pwrites=[] if c == 0 else [B_aT])
        for c in range(4):
            ri = load_piece(d_ap[c * 128:(c + 1) * 128, :], False)
            for nb in range(4):
                S.op("pe", lambda e, c=c, nb=nb, ri=ri: e.matmul(Pb[4 + nb][0:M, :], lhsT=aT[:, c, 0:M], rhs=ring[ri][:, nb * 512:(nb + 1) * 512], start=(c == 0), stop=(c == 3)),
                     reads=[B_aT, B_ring[ri]], writes=[Bk[4 + nb]] if c == 0 else [], pwrites=[] if c == 0 else [Bk[4 + nb]])

    BIGOOB = float(NSLOT + 4096)
    regc = {}

    def bcreg(e):
        if 'r' not in regc:
            regc['r'] = e.to_reg(ne * CAP - 1)
        return regc['r']

    for j in range(NOWN):
        hj = ht[j % 2]; Bhj = B_ht[j % 2]
        S.op("sp", lambda e, j=j: e.dma_start(out=xo, in_=xown_d[j * 128:(j + 1) * 128, :]), after=B_M if j == 0 else (), writes=[B_xo], dma=True)
        for hb in range(16):
            ri = load_piece(wout_d[hb * 128:(hb + 1) * 128, :], False)
            for nb in range(4):
                S.op("pe", lambda e, hb=hb, nb=nb, ri=ri, j=j: e.matmul(Pb[nb], lhsT=mixT[:, hb, j * 128:(j + 1) * 128], rhs=ring[ri][:, nb * 512:(nb + 1) * 512], start=(hb == 0), stop=(hb == 15)),
                     reads=[B_mixT, B_ring[ri]], writes=[Bk[nb]] if hb == 0 else [], pwrites=[] if hb == 0 else [Bk[nb]])
        for nb in range(4):
            S.op("dve", lambda e, nb=nb, hj=hj: e.scalar_tensor_tensor(hj[:, nb * 512:(nb + 1) * 512], xo[:, nb * 512:(nb + 1) * 512], ALPHA, Pb[nb], ALU.mult, ALU.add),
                 reads=[B_xo, Bk[nb]], after=B_M if j < 2 else (), writes=[Bhj] if nb == 0 else [], pwrites=[] if nb == 0 else [Bhj])
        layer_norm(hj, Bhj, ln_g, ln_b)
        if debug:
            S.op("sp", lambda e, j=j, hj=hj: e.dma_start(out=dbg["h"][j * 128:(j + 1) * 128, :], in_=hj), reads=[Bhj], dma=True)
        S.op("act", lambda e, hj=hj: e.copy(hbf, hj), reads=[Bhj], after=B_M if j == 0 else (), writes=[B_hbf])
        for half in range(2):
            for kk in range(8):
                k = half * 8 + kk
                S.op("pe", lambda e, k=k, kk=kk, half=half: e.transpose(Pbf[4 + half][:, kk * 128:(kk + 1) * 128], hbf[:, k * 128:(k + 1) * 128], ident_bf),
                     reads=[B_hbf, B_const], writes=[Bk[4 + half]] if kk == 0 else [], pwrites=[] if kk == 0 else [Bk[4 + half]])
            S.op("act", lambda e, half=half: e.copy(hT[:, half * 8:(half + 1) * 8, :].rearrange("p a b -> p (a b)"), Pbf[4 + half]), reads=[Bk[4 + half]],
                 after=B_M if j == 0 else (), writes=[B_hT] if half == 0 else [], pwrites=[] if half == 0 else [B_hT])
        for k in range(16):
            S.op("pe", lambda e, k=k: e.matmul(Pb[6][:, 0:256], lhsT=hT[:, k, :], rhs=wr_bf[:, k, :], start=(k == 0), stop=(k == 15)),
                 reads=[B_hT, B_wr], writes=[Bk[6]] if k == 0 else [], pwrites=[] if k == 0 else [Bk[6]])
        sc, Bsc = rt["sc"]; sel, Bsel = rt["sel"]; selm, Bselm = rt["selm"]; em, Bem = rt["em"]; gd, Bgd = rt["gd"]; csb, Bcsb = rt["csb"]
        S.op("act", lambda e: e.activation(sc, Pb[6][:, 0:256], AF.Sigmoid), reads=[Bk[6]], after=B_M if j == 0 else (), writes=[Bsc])
        S.op("dve", lambda e: e.tensor_tensor(sel, sc, rb_bc, ALU.add), reads=[Bsc, B_rb], writes=[Bsel])
        for g_ in range(8):
            S.op("dve", lambda e, g_=g_: e.max(gm[:, g_, :], sel[:, g_ * 32:(g_ + 1) * 32]), reads=[Bsel], writes=[B_r] if g_ == 0 else [], pwrites=[] if g_ == 0 else [B_r])
        S.op("dve", lambda e: e.tensor_tensor(gsum, gm[:, :, 0], gm[:, :, 1], ALU.add), reads=[B_r], pwrites=[B_r])
        S.op("dve", lambda e: e.max(m2, gsum), reads=[B_r], pwrites=[B_r])
        S.op("dve", lambda e: e.tensor_scalar(gmask, gsum, m2[:, 3:4], None, ALU.is_ge), reads=[B_r], pwrites=[B_r])
        S.op("dve", lambda e: e.tensor_scalar(pen, gmask, -1.0, 1e9, ALU.add, ALU.mult), reads=[B_r], pwrites=[B_r])
        for g_ in range(8):
            S.op("dve", lambda e, g_=g_: e.tensor_scalar(selm[:, g_ * 32:(g_ + 1) * 32], sel[:, g_ * 32:(g_ + 1) * 32], gmask[:, g_:g_ + 1], pen[:, g_:g_ + 1], ALU.mult, ALU.add),
                 reads=[Bsel, B_r], writes=[Bselm] if g_ == 0 else [], pwrites=[] if g_ == 0 else [Bselm])
        S.op("dve", lambda e: e.max(m8, selm), reads=[Bselm], pwrites=[B_r])
        S.op("dve", lambda e: e.tensor_scalar(em, selm, m8[:, 7:8], None, ALU.is_ge), reads=[Bselm, B_r], writes=[Bem])
        S.op("dve", lambda e, j=j: e.tensor_copy(em_bf[:, j, :], em), reads=[Bem], after=B_M if j == 0 else (), pwrites=[B_em])
        S.op("dve", lambda e: e.scalar_tensor_tensor(gd, sc, 1.0, em, ALU.mult, ALU.mult, accum_out=ssum[:, 0:1]), reads=[Bsc, Bem], writes=[Bgd], pwrites=[B_r])
        S.op("dve", lambda e: e.reciprocal(ssum[:, 1:2], ssum[:, 0:1]), reads=[B_r], pwrites=[B_r])
        S.op("dve", lambda e: e.tensor_scalar(gd, gd, ssum[:, 1:2], 2.5, ALU.mult, ALU.mult), reads=[B_r], writes=[Bgd])
        S.op("dve", lambda e: e.max(mk, gd), reads=[Bgd], pwrites=[B_r])
        S.op("dve", lambda e: e.max_index(idxu, mk, gd), reads=[Bgd, B_r], pwrites=[B_r])
        S.op("dve", lambda e: e.tensor_copy(idxf, idxu), reads=[B_r], pwrites=[B_r])
        S.op("pe", lambda e, j=j: e.matmul(Pb[7][:, 0:256], lhsT=ustrict_bf, rhs=em_bf[:, j, :], start=True, stop=(j == 0)), reads=[B_em, B_const], writes=[Bk[7]])
        for jj in range(j):
            S.op("pe", lambda e, jj=jj, j=j: e.matmul(Pb[7][:, 0:256], lhsT=ones_bf, rhs=em_bf[:, jj, :], start=False, stop=(jj == j - 1)), reads=[B_em, B_const], pwrites=[Bk[7]])
        S.op("act", lambda e: e.copy(csb, Pb[7][:, 0:256]), reads=[Bk[7]], writes=[Bcsb])
        for k in range(8):
            S.op("dve", lambda e, k=k: e.scalar_tensor_tensor(sel, iota_f, idxf[:, k:k + 1], csb, ALU.is_equal, ALU.mult, accum_out=posf[:, k:k + 1]),
                 reads=[Bcsb, B_r, B_const], writes=[Bsel], pwrites=[B_r])
        S.op("dve", lambda e: e.scalar_tensor_tensor(slotf, idxf, float(CAP), posf, ALU.mult, ALU.add), reads=[B_r], pwrites=[B_r])
        S.op("dve", lambda e: e.tensor_single_scalar(okf, posf, float(CAP), ALU.is_lt), reads=[B_r], pwrites=[B_r])
        if ne < NE:
            S.op("dve", lambda e: e.tensor_single_scalar(slotf, idxf, float(ne), ALU.is_lt), reads=[B_r], pwrites=[B_r])
            S.op("dve", lambda e: e.tensor_tensor(okf, okf, slotf, ALU.mult), reads=[B_r], pwrites=[B_r])
            S.op("dve", lambda e: e.scalar_tensor_tensor(slotf, idxf, float(CAP), posf, ALU.mult, ALU.add), reads=[B_r], pwrites=[B_r])
        S.op("dve", lambda e: e.scalar_tensor_tensor(slotf, slotf, -BIGOOB, okf, ALU.add, ALU.mult), reads=[B_r], pwrites=[B_r])
        S.op("dve", lambda e: e.tensor_scalar(slotf, slotf, BIGOOB, None, ALU.add), reads=[B_r], pwrites=[B_r])
        S.op("dve", lambda e, j=j: e.tensor_copy(slot_all[:, j, :], slotf), reads=[B_r], after=B_M if j == 0 else (), pwrites=[B_slot])
        S.op("dve", lambda e, j=j: e.tensor_tensor(wk_all[:, j, :], mk, okf, ALU.mult), reads=[B_r], after=B_M if j == 0 else (), pwrites=[B_wk])
        if debug:
            S.op("dve", lambda e: e.tensor_copy(sel[:, 0:8], idxf), reads=[B_r], writes=[Bsel])
            S.op("dve", lambda e, j=j: e.tensor_copy(sel[:, 8:16], wk_all[:, j, :]), reads=[B_wk], pwrites=[Bsel])
            S.op("sp", lambda e, j=j: e.dma_start(out=dbg["rt"][j * 128:(j + 1) * 128, :], in_=sel[:, 0:16]), reads=[Bsel], dma=True)
        for k in range(8):
            S.op("pool", lambda e, j=j, k=k: e.indirect_dma_start(out=xbuf_d[0:ne * CAP, :], out_offset=bass.IndirectOffsetOnAxis(ap=slot_all[:, j, k:k + 1], axis=0),
                                                              in_=hbf, in_offset=None, bounds_check=bcreg(e), oob_is_err=False),
                 reads=[B_hbf, B_slot], pwrites=[B_xbuf], after=[B_xbuf] if (j == 0 and k == 0) else (), dma=True)
        ffn(lambda k: hT[:, k, :], B_hT, 128, wsg_d, wsu_d, wsd_d)
        for nb in range(4):
            S.op("dve", lambda e, nb=nb, hj=hj: e.scalar_tensor_tensor(hj[:, nb * 512:(nb + 1) * 512], hj[:, nb * 512:(nb + 1) * 512], ALPHA, Pb[4 + nb], ALU.mult, ALU.add),
                 reads=[Bk[4 + nb]], writes=[Bhj])
        S.op("sp", lambda e, j=j, hj=hj: e.dma_start(out=accb_d[j * 128:(j + 1) * 128, :], in_=hj), reads=[Bhj], pwrites=[B_accb], dma=True)

    ring_active[0] = NRING + NEXTRA
    for ex in range(ne):
        S.op("sp", lambda e, ex=ex: e.dma_start(out=xe[0:CAP, :], in_=xbuf_d[ex * CAP:(ex + 1) * CAP, :]), reads=[B_xbuf], writes=[B_xe], dma=True)
        for k in range(16):
            S.op("pe", lambda e, k=k: e.transpose(Pbf[3][:, k * CAP:(k + 1) * CAP], xe[0:CAP, k * 128:(k + 1) * 128], ident_bf[0:CAP, 0:CAP]),
                 reads=[B_xe, B_const], writes=[Bk[3]] if k == 0 else [], pwrites=[] if k == 0 else [Bk[3]])
        S.op("act", lambda e: e.copy(xeT.rearrange("p a b -> p (a b)"), Pbf[3]), reads=[Bk[3]], writes=[B_xeT])
        ffn(lambda k: xeT[:, k, :], B_xeT, CAP, wg_d[ex], wu_d[ex], wd_d[ex])
        for nb in range(4):
            S.op("act", lambda e, nb=nb: e.copy(ysb[0:CAP, nb * 512:(nb + 1) * 512], Pb[4 + nb][0:CAP, :]), reads=[Bk[4 + nb]], writes=[B_ysb] if nb == 0 else [], pwrites=[] if nb == 0 else [B_ysb])
        S.op("sp", lambda e, ex=ex: e.dma_start(out=ybuf_d[ex * CAP:(ex + 1) * CAP, :], in_=ysb[0:CAP, :]), reads=[B_ysb], pwrites=[B_ybuf], dma=True)

    S.op("sp", lambda e: e.dma_start(out=ln_g, in_=bc(ln_d[2])), after=B_ring, writes=[B_ln], dma=True)
    S.op("sp", lambda e: e.dma_start(out=ln_b, in_=bc(ln_d[3])), pwrites=[B_ln], dma=True)
    yg = ring
    yg_t = [sb(R0 + i * 8192, [128, 2048], F32) for i in range(2)]
    B_yg = [Buf(), Buf()]
    for i in range(2):
        S.op("dve", lambda e, i=i: e.memset(yg_t[i], 0.0), after=B_ring, writes=[B_yg[i]])
    gi = 0
    for j in range(NOWN):
        hj = ht[j % 2]; Bhj = B_ht[j % 2]
        S.op("sp", lambda e, j=j, hj=hj: e.dma_start(out=hj, in_=accb_d[j * 128:(j + 1) * 128, :]), reads=[B_accb], after=B_ring if j < 2 else (), writes=[Bhj], dma=True)
        for k in range(8):
            t_ = yg_t[gi % 2]; Bt_ = B_yg[gi % 2]; gi += 1
            S.op("pool", lambda e, j=j, k=k, t_=t_: e.indirect_dma_start(out=t_, out_offset=None, in_=ybuf_d[0:ne * CAP, :],
                                                                        in_offset=bass.IndirectOffsetOnAxis(ap=slot_all[:, j, k:k + 1], axis=0),
                                                                        bounds_check=bcreg(e), oob_is_err=False),
                 reads=[B_ybuf, B_slot], writes=[Bt_], dma=True)
            S.op("dve", lambda e, j=j, k=k, t_=t_, hj=hj: e.scalar_tensor_tensor(hj, t_, wk_all[:, j, k:k + 1], hj, ALU.mult, ALU.add),
                 reads=[Bt_, B_wk], writes=[Bhj])
        if debug:
            S.op("sp", lambda e, j=j, hj=hj: e.dma_start(out=dbg["acc"][j * 128:(j + 1) * 128, :], in_=hj), reads=[Bhj], dma=True)
        layer_norm(hj, Bhj, ln_g, ln_b)
        S.op("sp", lambda e, j=j, hj=hj: e.dma_start(out=out_d[j * 128:(j + 1) * 128, :], in_=hj), reads=[Bhj], dma=True)
    return finish(nc, S, es, esem, dsem)


def finish(nc, S, es, esem, dsem):
    final = set(t for t in S.dma_last if t is not None)
    S.ops["sp"].append((None, final, None))
    with nc.Block() as block:
        S.emit(nc, block, esem, dsem)
    es.close()
    return nc


def host_layout(inputs, ne=NE):
    x = np.asarray(inputs["x"], np.float32)
    w_in = np.asarray(inputs["w_in"], np.float32)[0]
    secs = [w_in[:, i * 1024:(i + 1) * 1024] for i in range(8)]
    blocks = []
    for h in range(8):
        blocks.append(np.concatenate([secs[i][:, h * 128:(h + 1) * 128] for i in (0, 1, 2, 3)], axis=1))
    for h in range(8):
        blocks.append(np.concatenate([secs[i][:, h * 128:(h + 1) * 128] for i in (4, 5, 6, 7)], axis=1))
    w_in_h = np.ascontiguousarray(np.stack(blocks, 0))
    ln_par = np.ascontiguousarray(np.stack([inputs["ln1_gain"][0], inputs["ln1_bias"][0],
                                            inputs["ln2_gain"][0], inputs["ln2_bias"][0]], 0).astype(np.float32))
    shared = {
        "w_in_h": w_in_h,
        "w_out": np.ascontiguousarray(inputs["w_out"][0], np.float32),
        "ret_gain": np.ascontiguousarray(inputs["ret_gn_gain"][0], np.float32),
        "hg_gain": np.ascontiguousarray(inputs["hgrn_norm_gain"][0], np.float32),
        "lb_logits": np.ascontiguousarray(inputs["hgrn_lb_logits"], np.float32),
        "ln_par": ln_par,
        "w_router": np.ascontiguousarray(inputs["w_router"][0], np.float32),
        "router_bias": np.ascontiguousarray(inputs["router_bias"][0], np.float32),
        "w_gate": np.asarray(inputs["w_gate"][0][:ne], np.float32),
        "w_up": np.asarray(inputs["w_up"][0][:ne], np.float32),
        "w_down": np.asarray(inputs["w_down"][0][:ne], np.float32),
        "ws_gate": np.ascontiguousarray(inputs["ws_gate"][0], np.float32),
        "ws_up": np.ascontiguousarray(inputs["ws_up"][0], np.float32),
        "ws_down": np.ascontiguousarray(inputs["ws_down"][0], np.float32),
    }
    maps = []
    for c in range(8):
        b, th = c // 2, c % 2
        if th == 1:
            xT = np.ascontiguousarray(x[b].T)
        else:
            xT = np.ascontiguousarray(np.concatenate([np.zeros((D, 1024), np.float32), x[b, 0:1024].T], axis=1))
        m = dict(shared)
        m["xT"] = xT
        m["xown"] = np.ascontiguousarray(x[b, th * 1024:(th + 1) * 1024])
        m["posb"] = np.full((128, 1), 0.0 if th == 1 else -1024.0, np.float32)
        maps.append(m)
    return maps


def kernel(**inputs):
    maps = host_layout(inputs, ne=NE)
    nc = build(ne=NE, debug=False)
    res = run_bass_kernel_spmd(nc, maps, core_ids=list(range(8)))
    out = np.zeros((4, 2048, 2048), np.float32)
    for c in range(8):
        b, th = c // 2, c % 2
        out[b, th * 1024:(th + 1) * 1024] = np.asarray(res.results[c]["out"], np.float32)
    return out
```
